# Optimizing a Trainium2 kernel written in Bass

```python
import jax, jax.numpy as jnp
from jax import lax
import numpy as np

D_MODEL = 1024
BATCH = 16
SEQ = 2048
DEPTH = 2

GRID_W = 64
CTX_LEN = 256
MIX_WIDTH = D_MODEL
RET_WIDTH = MIX_WIDTH // 2
POOL_WIDTH = MIX_WIDTH - RET_WIDTH
RET_HEADS = 4
RET_HEAD_DIM = RET_WIDTH // RET_HEADS
RET_CHUNK = 128
POOL_WINDOWS = (2, 4, 8, 16)
POOL_GROUPS = len(POOL_WINDOWS)
POOL_GROUP_DIM = POOL_WIDTH // POOL_GROUPS
IN_PROJ_WIDTH = 4 * RET_WIDTH + POOL_WIDTH
IN_PROJ_SPLITS = (RET_WIDTH, 2 * RET_WIDTH, 3 * RET_WIDTH, 4 * RET_WIDTH)
N_GROUPS = 4
EXPERTS_PER_GROUP = 8
N_EXPERTS = N_GROUPS * EXPERTS_PER_GROUP
TOP_K = 2
EXPERT_FF = D_MODEL // 2
MOE_BLOCK = 128
ROPE_BASE = 10000.0
NORM_EPS = 1e-6

kernel_name = "hybrid_retention_pool_hmoe_dit"


def rms_norm(x, gain):
    xf = x.astype(jnp.float32)
    y = xf * lax.rsqrt(jnp.mean(xf * xf, axis=-1, keepdims=True) + NORM_EPS)
    return (y * gain.astype(jnp.float32)).astype(x.dtype)


def modulate(x, gain, shift, scale):
    return rms_norm(x, gain) * (1.0 + scale) + shift


def rope_1d(x, pos):
    m = x.shape[-1] // 2
    inv = ROPE_BASE ** (-jnp.arange(m, dtype=jnp.float32) / m)
    ang = pos.astype(jnp.float32)[:, None] * inv[None, :]
    cos, sin = jnp.cos(ang), jnp.sin(ang)
    x1, x2 = x[..., :m], x[..., m:]
    return jnp.concatenate([x1 * cos - x2 * sin, x1 * sin + x2 * cos], axis=-1)


def rope_2d(x, rows, cols):
    h = x.shape[-1] // 2
    return jnp.concatenate([rope_1d(x[..., :h], rows), rope_1d(x[..., h:], cols)], axis=-1)


def to_heads(a):
    B, T, _ = a.shape
    return a.astype(jnp.float32).reshape(B, T, RET_HEADS, RET_HEAD_DIM).transpose(0, 2, 1, 3)


def retention_chunkwise(q, k, v, log_gamma, s0):
    B, H, T, Dh = q.shape
    C = RET_CHUNK
    n_chunks = T // C
    pos = jnp.arange(C, dtype=jnp.float32)
    diff = pos[:, None] - pos[None, :]
    intra = jnp.where(diff >= 0.0, jnp.exp(log_gamma[:, None, None] * jnp.maximum(diff, 0.0)), 0.0)
    q_decay = jnp.exp(log_gamma[:, None] * (pos + 1.0))[None, :, :, None]
    k_decay = jnp.exp(log_gamma[:, None] * (C - 1.0 - pos))[None, :, :, None]
    chunk_decay = jnp.exp(log_gamma * C)[None, :, None, None]

    def chunks(a):
        return a.reshape(B, H, n_chunks, C, Dh).transpose(2, 0, 1, 3, 4)

    def step(state, qkv):
        qc, kc, vc = qkv
        scores = jnp.einsum('bhid,bhjd->bhij', qc, kc) * intra
        o = jnp.einsum('bhij,bhjd->bhid', scores, vc) + jnp.einsum('bhid,bhde->bhie', qc * q_decay, state)
        state = state * chunk_decay + jnp.einsum('bhjd,bhje->bhde', kc * k_decay, vc)
        return state, o

    s_final, o = lax.scan(step, s0, (chunks(q), chunks(k), chunks(v)))
    return o.transpose(1, 2, 0, 3, 4).reshape(B, H, T, Dh), s_final


def bidir_retention(q_c, k_c, v_c, q_l, k_l, v_l, lg_f, lg_b):
    B, H, _, Dh = q_c.shape
    flip = lambda a: jnp.flip(a, axis=2)
    zero = jnp.zeros((B, H, Dh, Dh), jnp.float32)
    o_cf, s_f = retention_chunkwise(q_c, k_c, v_c, lg_f, zero)
    o_cb, s_b = retention_chunkwise(flip(q_c), flip(k_c), flip(v_c), lg_b, zero)
    o_lf, _ = retention_chunkwise(q_l, k_l, v_l, lg_f, s_f)
    o_lb, _ = retention_chunkwise(flip(q_l), flip(k_l), flip(v_l), lg_b, s_b)
    return o_cf + flip(o_cb), o_lf + flip(o_lb)


def multiscale_pool(p, pool_w, pool_scale):
    B, T, _ = p.shape
    grp = p.astype(jnp.float32).reshape(B, T, POOL_GROUPS, POOL_GROUP_DIM)
    cs = jnp.concatenate([jnp.zeros((B, 1, POOL_GROUPS, POOL_GROUP_DIM), jnp.float32),
                          jnp.cumsum(grp, axis=1)], axis=1)
    win = jnp.array(POOL_WINDOWS, jnp.int32)
    t = jnp.arange(T, dtype=jnp.int32)[:, None]
    lo = jnp.clip(t - win // 2, 0, T)
    hi = jnp.clip(t + win // 2, 0, T)
    gidx = jnp.arange(POOL_GROUPS, dtype=jnp.int32)[None, :]
    wsum = cs[:, hi, gidx] - cs[:, lo, gidx]
    mean = wsum / (hi - lo).astype(jnp.float32)[None, :, :, None]
    out = jnp.einsum('btgc,gcd->btgd', mean - grp, pool_w.astype(jnp.float32))
    return out.reshape(B, T, POOL_WIDTH) * pool_scale.astype(jnp.float32)


def token_mixer(h_lat, h_ctx, w_in, decay_f, decay_b, pool_w, pool_scale, w_out, rows, cols, ctx_out):
    pl = h_lat @ w_in
    pc = h_ctx @ w_in
    ql, kl, vl, gl, xl = jnp.split(pl, IN_PROJ_SPLITS, axis=-1)
    qc, kc, vc, gc, xc = jnp.split(pc, IN_PROJ_SPLITS, axis=-1)
    k_scale = RET_HEAD_DIM ** -0.5
    ql = rope_2d(to_heads(ql), rows, cols)
    kl = rope_2d(to_heads(kl), rows, cols) * k_scale
    qc = to_heads(qc)
    kc = to_heads(kc) * k_scale
    lg_f = jax.nn.log_sigmoid(decay_f.astype(jnp.float32))
    lg_b = jax.nn.log_sigmoid(decay_b.astype(jnp.float32))
    o_c, o_l = bidir_retention(qc, kc, to_heads(vc), ql, kl, to_heads(vl), lg_f, lg_b)

    def merge(o, g, xp):
        B, H, T, Dh = o.shape
        o = o * lax.rsqrt(jnp.mean(o * o, axis=-1, keepdims=True) + NORM_EPS)
        o = o.transpose(0, 2, 1, 3).reshape(B, T, RET_WIDTH)
        ret = jax.nn.silu(g.astype(jnp.float32)) * o
        pool = multiscale_pool(xp, pool_w, pool_scale)
        return jnp.concatenate([ret, pool], axis=-1).astype(h_lat.dtype) @ w_out

    y_lat = merge(o_l, gl, xl)
    y_ctx = merge(o_c, gc, xc) if ctx_out else None
    return y_lat, y_ctx


def hier_moe(h, rg_w, rg_b, re_w, re_b, w_gate, w_up, w_down):
    T, D = h.shape
    hf = h.astype(jnp.float32)
    g_logits = hf @ rg_w.astype(jnp.float32) + rg_b.astype(jnp.float32)
    g_top = jnp.argmax(g_logits, axis=-1)
    g_w = jnp.take_along_axis(jax.nn.softmax(g_logits, axis=-1), g_top[:, None], axis=-1)
    e_logits = (hf @ re_w.astype(jnp.float32) + re_b.astype(jnp.float32)).reshape(T, N_GROUPS, EXPERTS_PER_GROUP)
    e_in = jnp.take_along_axis(e_logits, g_top[:, None, None], axis=1)[:, 0]
    top_vals, top_idx = lax.top_k(e_in, TOP_K)
    top_w = jax.nn.softmax(top_vals, axis=-1) * g_w
    expert_idx = g_top[:, None].astype(jnp.int32) * EXPERTS_PER_GROUP + top_idx.astype(jnp.int32)

    A = T * TOP_K
    e_flat = expert_idx.reshape(A)
    w_flat = top_w.reshape(A)
    tok_flat = jnp.repeat(jnp.arange(T, dtype=jnp.int32), TOP_K)
    order = jnp.argsort(e_flat)
    e_sorted = e_flat[order]
    counts = jnp.bincount(e_flat, length=N_EXPERTS)
    starts = jnp.cumsum(counts) - counts
    padded = (counts + MOE_BLOCK - 1) // MOE_BLOCK * MOE_BLOCK
    pends = jnp.cumsum(padded)
    pstarts = pends - padded
    dest = pstarts[e_sorted] + (jnp.arange(A, dtype=jnp.int32) - starts[e_sorted])
    n_blocks = -(-A // MOE_BLOCK) + N_EXPERTS
    n_rows = n_blocks * MOE_BLOCK
    buf_tok = jnp.full((n_rows,), T, jnp.int32).at[dest].set(tok_flat[order])
    buf_w = jnp.zeros((n_rows,), jnp.float32).at[dest].set(w_flat[order])
    block_start = jnp.arange(n_blocks, dtype=jnp.int32) * MOE_BLOCK
    block_expert = jnp.minimum(jnp.searchsorted(pends, block_start, side='right'), N_EXPERTS - 1)
    h_pad = jnp.concatenate([h, jnp.zeros((1, D), h.dtype)], axis=0)

    def run_block(args):
        tok, e = args
        xb = h_pad[tok]
        return (jax.nn.silu(xb @ w_gate[e]) * (xb @ w_up[e])) @ w_down[e]

    y_buf = lax.map(run_block, (buf_tok.reshape(n_blocks, MOE_BLOCK), block_expert))
    y_buf = y_buf.reshape(n_rows, D).astype(jnp.float32) * buf_w[:, None]
    return jax.ops.segment_sum(y_buf, buf_tok, num_segments=T + 1)[:T].astype(h.dtype)


def setup_inputs(seed: int = 0) -> dict:
    key = jax.random.key(seed)
    ks = jax.random.split(key, 24)
    f32 = jnp.float32

    def nrm(k, shape, scale):
        return jax.random.normal(k, shape, f32) * scale

    gamma0 = 1.0 - 2.0 ** (-5.0 - np.arange(RET_HEADS, dtype=np.float32))
    logit0 = jnp.asarray(np.log(gamma0 / (1.0 - gamma0)), f32)
    return {
        "x": nrm(ks[0], (BATCH, SEQ, D_MODEL), 1.0),
        "c": nrm(ks[1], (BATCH, D_MODEL), 1.0),
        "ctx": nrm(ks[2], (BATCH, CTX_LEN, D_MODEL), 1.0),
        "c_ctx": nrm(ks[3], (D_MODEL,), 1.0),
        "ada_w": nrm(ks[4], (DEPTH, D_MODEL, 6 * D_MODEL), 0.5 * D_MODEL ** -0.5),
        "ada_b": nrm(ks[5], (DEPTH, 6 * D_MODEL), 0.02),
        "norm1_g": 1.0 + nrm(ks[6], (DEPTH, D_MODEL), 0.1),
        "w_in": nrm(ks[7], (DEPTH, D_MODEL, IN_PROJ_WIDTH), D_MODEL ** -0.5),
        "decay_fwd": logit0[None, :] + nrm(ks[8], (DEPTH, RET_HEADS), 0.05),
        "decay_bwd": logit0[None, :] + nrm(ks[9], (DEPTH, RET_HEADS), 0.05),
        "pool_w": nrm(ks[10], (DEPTH, POOL_GROUPS, POOL_GROUP_DIM, POOL_GROUP_DIM), POOL_GROUP_DIM ** -0.5),
        "pool_scale": 1.0 + nrm(ks[11], (DEPTH, POOL_WIDTH), 0.1),
        "w_out": nrm(ks[12], (DEPTH, MIX_WIDTH, D_MODEL), MIX_WIDTH ** -0.5),
        "norm2_g": 1.0 + nrm(ks[13], (DEPTH, D_MODEL), 0.1),
        "router_g_w": nrm(ks[14], (DEPTH, D_MODEL, N_GROUPS), D_MODEL ** -0.5),
        "router_g_b": nrm(ks[15], (DEPTH, N_GROUPS), 0.01),
        "router_e_w": nrm(ks[16], (DEPTH, D_MODEL, N_EXPERTS), D_MODEL ** -0.5),
        "router_e_b": nrm(ks[17], (DEPTH, N_EXPERTS), 0.01),
        "exp_w_gate": nrm(ks[18], (DEPTH, N_EXPERTS, D_MODEL, EXPERT_FF), D_MODEL ** -0.5),
        "exp_w_up": nrm(ks[19], (DEPTH, N_EXPERTS, D_MODEL, EXPERT_FF), D_MODEL ** -0.5),
        "exp_w_down": nrm(ks[20], (DEPTH, N_EXPERTS, EXPERT_FF, D_MODEL), EXPERT_FF ** -0.5),
        "final_norm_g": 1.0 + nrm(ks[21], (D_MODEL,), 0.1),
    }


def reference(x, c, ctx, c_ctx, ada_w, ada_b, norm1_g, w_in, decay_fwd, decay_bwd, pool_w, pool_scale,
              w_out, norm2_g, router_g_w, router_g_b, router_e_w, router_e_b, exp_w_gate, exp_w_up,
              exp_w_down, final_norm_g):
    B, T, D = x.shape
    rows_n = T // GRID_W
    rows = jnp.repeat(jnp.arange(rows_n, dtype=jnp.int32), GRID_W)
    cols = jnp.tile(jnp.arange(GRID_W, dtype=jnp.int32), rows_n)
    ctx_h = ctx
    n_ctx = ctx.shape[1]
    for l in range(DEPTH):
        last = l == DEPTH - 1
        mod = jax.nn.silu(c) @ ada_w[l] + ada_b[l]
        mod_c = jax.nn.silu(c_ctx) @ ada_w[l] + ada_b[l]
        sh1, sc1, g1, sh2, sc2, g2 = jnp.split(mod[:, None, :], 6, axis=-1)
        csh1, csc1, cg1, csh2, csc2, cg2 = jnp.split(mod_c[None, None, :], 6, axis=-1)

        h_lat = modulate(x, norm1_g[l], sh1, sc1)
        h_ctx = modulate(ctx_h, norm1_g[l], csh1, csc1)
        y_lat, y_ctx = token_mixer(h_lat, h_ctx, w_in[l], decay_fwd[l], decay_bwd[l], pool_w[l], pool_scale[l],
                                   w_out[l], rows, cols, not last)
        x = x + g1 * y_lat
        moe_args = (router_g_w[l], router_g_b[l], router_e_w[l], router_e_b[l],
                    exp_w_gate[l], exp_w_up[l], exp_w_down[l])
        h_lat = modulate(x, norm2_g[l], sh2, sc2)
        if not last:
            ctx_h = ctx_h + cg1 * y_ctx
            h_ctx = modulate(ctx_h, norm2_g[l], csh2, csc2)
            tokens = jnp.concatenate([h_lat.reshape(B * T, D), h_ctx.reshape(B * n_ctx, D)], axis=0)
            y = hier_moe(tokens, *moe_args)
            x = x + g2 * y[:B * T].reshape(B, T, D)
            ctx_h = ctx_h + cg2 * y[B * T:].reshape(B, n_ctx, D)
        else:
            y = hier_moe(h_lat.reshape(B * T, D), *moe_args)
            x = x + g2 * y.reshape(B, T, D)
    return rms_norm(x, final_norm_g)
```

```python
import os
import numpy as np
from contextlib import ExitStack
import concourse.bass as bass
import concourse.mybir as mybir
from concourse.bass_utils import run_bass_kernel_spmd

F32 = mybir.dt.float32
BF16 = mybir.dt.bfloat16
I32 = mybir.dt.int32
AF = mybir.ActivationFunctionType
ALU = mybir.AluOpType
AX = mybir.AxisListType

D = 1024
T = 2048
NCX = 256
TT = T + NCX
NT = TT // 128
NB = 2
DEPTH = 2
NE = 32
NBLK = 108
NROWS = NBLK * 128
NSTREAM = 6
BPS = NBLK // NSTREAM
BIGIDX = 1 << 20
EPS = 1e-6
KS = 128.0 ** -0.5


class V:
    __slots__ = ("ap", "key")

    def __init__(self, ap, key):
        self.ap = ap
        self.key = key

    def __getitem__(self, idx):
        return V(self.ap[idx], self.key)

    def sub(self, idx, key):
        return V(self.ap[idx], key)

    def re(self, pat, **kw):
        return V(self.ap.rearrange(pat, **kw), self.key)

    def bc(self, axis, shape):
        return V(self.ap.unsqueeze(axis).to_broadcast(list(shape)), self.key)


class Sched:
    def __init__(self, nc, stack, n_dma_sems=8):
        self.nc = nc
        self.eng = {'pe': nc.tensor, 'dve': nc.vector, 'act': nc.scalar, 'pool': nc.gpsimd, 'sp': nc.sync}
        self.sem = {}
        self.cnt = {}
        self.waited = {e: {} for e in self.eng}
        for e in self.eng:
            self.sem[e] = stack.enter_context(nc.semaphore(f"s_{e}"))
            self.cnt[e] = 0
        self.dsem = {}
        self.dval = {}
        self.dnext = {}
        for q, nq in (('sp', 12), ('pool', 24)):
            self.dsem[q] = [stack.enter_context(nc.semaphore(f"d_{q}{i}")) for i in range(nq)]
            self.dval[q] = [0] * nq
            self.dnext[q] = 0
        self.last_w = {}
        self.readers = {}
        self.n_ins = 0
        self.excl = set()

    def _deps(self, reads, writes):
        deps = []
        for k in reads:
            if k in self.last_w:
                deps.append(self.last_w[k])
        for k in writes:
            if k in self.last_w:
                deps.append(self.last_w[k])
            deps.extend(self.readers.get(k, ()))
        return deps

    def _wait(self, e, deps):
        w = self.waited[e]
        need = {}
        for (sid, sem, val) in deps:
            if e == 'pe' and sid == 'c_pe':
                continue
            if w.get(sid, 0) >= val:
                continue
            if sid not in need or need[sid][1] < val:
                need[sid] = (sem, val)
        for sid, (sem, val) in need.items():
            self.eng[e].wait_ge(sem, val)
            w[sid] = val
            self.n_ins += 1

    def _commit(self, tok, reads, writes):
        for k in reads:
            self.readers.setdefault(k, []).append(tok)
        for k in writes:
            self.last_w[k] = tok
            self.readers[k] = []

    def op(self, e, fn, R=(), W=()):
        W = [k for k in W if k is not None] + [k for k in R if k in self.excl]
        R = [k for k in R if k is not None and k not in self.excl]
        self._wait(e, self._deps(R, W))
        ins = fn(self.eng[e])
        self.cnt[e] += 1
        ins.then_inc(self.sem[e], 1)
        tok = ('c_' + e, self.sem[e], self.cnt[e])
        self._commit(tok, R, W)
        self.n_ins += 1
        return tok

    def dma(self, q, fn, R=(), W=()):
        R = [k for k in R if k is not None]
        W = [k for k in W if k is not None]
        deps = self._deps(R, W)
        i = self.dnext[q]
        self.dnext[q] = (i + 1) % len(self.dsem[q])
        sem = self.dsem[q][i]
        sid = f'd_{q}{i}'
        if self.dval[q][i] > 0:
            deps.append((sid, sem, self.dval[q][i]))
        self._wait(q, deps)
        ins = fn(self.eng[q])
        self.dval[q][i] += 16
        ins.then_inc(sem, 16)
        tok = (sid, sem, self.dval[q][i])
        self._commit(tok, R, W)
        self.n_ins += 1
        return tok

    def all_tokens(self):
        toks = []
        for e in self.eng:
            if self.cnt[e] > 0:
                toks.append(('c_' + e, self.sem[e], self.cnt[e]))
        for q in self.dsem:
            for i, sem in enumerate(self.dsem[q]):
                if self.dval[q][i] > 0:
                    toks.append((f'd_{q}{i}', sem, self.dval[q][i]))
        return toks

    def barrier(self, engines=None):
        toks = self.all_tokens()
        for e in (engines or self.eng):
            self._wait(e, [t for t in toks if not (t[0] == 'c_' + e)])
        self.last_w = {}
        self.readers = {}

    def mm(self, out, lhsT, rhs, start=True, stop=True):
        return self.op('pe', lambda e: e.matmul(out.ap, lhsT=lhsT.ap, rhs=rhs.ap, start=start, stop=stop),
                       R=[lhsT.key, rhs.key], W=[out.key])

    def tr(self, out, in_, ident):
        return self.op('pe', lambda e: e.transpose(out.ap, in_.ap, ident.ap), R=[in_.key, ident.key], W=[out.key])

    def act(self, out, in_, func, bias=None, scale=None, accum=None, eng='act'):
        kw = {}
        R = [in_.key]
        W = [out.key]
        if bias is not None:
            if isinstance(bias, V):
                kw['bias'] = bias.ap
                R.append(bias.key)
            else:
                kw['bias'] = bias
        if scale is not None:
            if isinstance(scale, V):
                kw['scale'] = scale.ap
                R.append(scale.key)
            else:
                kw['scale'] = scale
        if accum is not None:
            kw['accum_out'] = accum.ap
            W.append(accum.key)
        return self.op('act', lambda e: e.activation(out=out.ap, in_=in_.ap, func=func, **kw), R=R, W=W)

    def tt(self, eng, out, in0, in1, op):
        return self.op(eng, lambda e: e.tensor_tensor(out=out.ap, in0=in0.ap, in1=in1.ap, op=op),
                       R=[in0.key, in1.key], W=[out.key])

    def ts(self, eng, out, in0, s1, op0, s2=None, op1=None, accum=None):
        R = [in0.key]
        W = [out.key]
        a1 = s1
        if isinstance(s1, V):
            a1 = s1.ap
            R.append(s1.key)
        a2 = s2
        if isinstance(s2, V):
            a2 = s2.ap
            R.append(s2.key)
        kw = {}
        if op1 is not None:
            kw['op1'] = op1
        if accum is not None:
            kw['accum_out'] = accum.ap
            W.append(accum.key)
        return self.op(eng, lambda e: e.tensor_scalar(out=out.ap, in0=in0.ap, scalar1=a1, scalar2=a2, op0=op0, **kw),
                       R=R, W=W)

    def stt(self, out, in0, scalar, in1, op0, op1):
        R = [in0.key, in1.key]
        a = scalar
        if isinstance(scalar, V):
            a = scalar.ap
            R.append(scalar.key)
        return self.op('dve', lambda e: e.scalar_tensor_tensor(out=out.ap, in0=in0.ap, scalar=a, in1=in1.ap,
                                                               op0=op0, op1=op1), R=R, W=[out.key])

    def cp(self, eng, out, in_):
        if eng == 'act':
            return self.op('act', lambda e: e.copy(out=out.ap, in_=in_.ap), R=[in_.key], W=[out.key])
        return self.op(eng, lambda e: e.tensor_copy(out=out.ap, in_=in_.ap), R=[in_.key], W=[out.key])

    def red(self, out, in_, op, axis=None):
        ax = axis if axis is not None else AX.X
        return self.op('dve', lambda e: e.tensor_reduce(out=out.ap, in_=in_.ap, axis=ax, op=op),
                       R=[in_.key], W=[out.key])

    def recip(self, out, in_):
        return self.op('dve', lambda e: e.reciprocal(out=out.ap, in_=in_.ap), R=[in_.key], W=[out.key])

    def memset(self, eng, out, val):
        return self.op(eng, lambda e: e.memset(out.ap, val), W=[out.key])

    def ld(self, out, src_ap, src_key=None, q='sp', **kw):
        return self.dma(q, lambda e: e.dma_start(out=out.ap, in_=src_ap, **kw), R=[src_key], W=[out.key])

    def st(self, dst_ap, dst_key, in_, q='sp', **kw):
        return self.dma(q, lambda e: e.dma_start(out=dst_ap, in_=in_.ap, **kw), R=[in_.key], W=[dst_key])


def pipeline(n_items, stages):
    leads = [ld for (_, ld) in stages]
    for t in range(-max(leads), n_items - min(leads)):
        for f, ld in stages:
            i = t + ld
            if 0 <= i < n_items:
                f(i)


class Ring:
    def __init__(self, views):
        self.views = views
        self.i = 0

    def next(self):
        v = self.views[self.i % len(self.views)]
        self.i += 1
        return v


def build(stop=None, dbg=False):
    nc = bass.Bass("TRN2", target_bir_lowering=False)

    def din(name, shape, dt=F32):
        return nc.dram_tensor(name, list(shape), dt, kind="ExternalInput").ap()

    def dscr(name, shape, dt, out=False):
        return nc.dram_tensor(name, list(shape), dt, kind=("ExternalOutput" if out else "Internal")).ap()

    x_in = din("x", [NB, T, D])
    ctx_in = din("ctx", [NB, NCX, D])
    cvec = din("cvec", [3, D])
    ada_w = din("ada_w", [DEPTH, D, 6 * D])
    ada_b = din("ada_b", [DEPTH, 6 * D])
    norm1_g = din("norm1_g", [DEPTH, D])
    w_in = din("w_in", [DEPTH, D, 2560])
    decay_f = din("decay_fwd", [DEPTH, 4])
    decay_b = din("decay_bwd", [DEPTH, 4])
    pool_w = din("pool_w", [DEPTH, 4, 128, 128])
    pool_scale = din("pool_scale", [DEPTH, 512])
    w_out = din("w_out", [DEPTH, D, D])
    norm2_g = din("norm2_g", [DEPTH, D])
    router_w = din("router_w", [DEPTH, D, 36])
    router_b = din("router_b", [DEPTH, 36])
    w_gate = din("exp_w_gate", [DEPTH, NE, D, 512])
    w_up = din("exp_w_up", [DEPTH, NE, D, 512])
    w_down = din("exp_w_down", [DEPTH, NE, 512, D])
    fin_g = din("final_norm_g", [D])
    c_rope_c = din("c_rope_c", [128, 16, 128])
    c_rope_s = din("c_rope_s", [128, 16, 128])
    c_band = din("c_band", [128, 20, 128])
    c_ident = din("c_ident", [128, 128])
    c_utri = din("c_utri", [128, 128])
    c_mtab = din("c_mtab", [128, 4, 128])
    c_pcols = din("c_pcols", [128, 8])
    c_blk = din("c_blk", [128, NBLK])

    out = nc.dram_tensor("out", [NB, T, D], F32, kind="ExternalOutput").ap()
    XS = dscr("XS", [NB, TT, D], F32, out=dbg)
    MOD = dscr("MOD", [DEPTH, 3, 6 * D], F32)
    GXKV = dscr("GXKV", [NB, TT, 2048], BF16)
    SBX = dscr("SBX", [NB, NT, 128, 512], BF16)
    HT = dscr("HT", [NB * TT, D], BF16)
    HB = dscr("HB", [NROWS, D], BF16)
    YB = dscr("YB", [NROWS, D], F32)
    WB2 = dscr("WB", [DEPTH, NE * 128, 12288], BF16)

    with ExitStack() as top:
        S = Sched(nc, top)
        bc_reg = nc.gpsimd.to_reg(DEPTH * NE * 128 - 1)

        uid = [0]

        def sb(st, name, shape, dt):
            uid[0] += 1
            nm = f"{name}_{uid[0]}"
            return V(st.enter_context(nc.sbuf_tensor(nm, list(shape), dt))[:], nm)

        def ps(st, name, shape, dt=F32):
            uid[0] += 1
            nm = f"{name}_{uid[0]}"
            S.excl.add(nm)
            return V(st.enter_context(nc.psum_tensor(nm, list(shape), dt))[:], nm)

        def ring(st, name, n, shape, dt, psum=False):
            f = ps if psum else sb
            return Ring([f(st, f"{name}{i}", shape, dt) for i in range(n)])

        ident_b = sb(top, "ident_b", [128, 128], BF16)
        ident_f = sb(top, "ident_f", [128, 128], F32)
        ones_f = sb(top, "ones_f", [128, 128], F32)
        pcols = sb(top, "pcols", [128, 8], F32)
        epsc = sb(top, "epsc", [128, 1], F32)
        S.ld(ident_f, c_ident[:, :])
        S.ld(ident_b, c_ident[:, :], q='pool')
        S.ld(pcols, c_pcols[:, :])
        S.memset('dve', ones_f, 1.0)
        S.memset('dve', epsc, EPS)

        def rstd_from_ssq(rstd, ssq, tmp, inv_n):
            S.act(tmp, ssq, AF.Sqrt, bias=epsc[0:ssq.ap.shape[0], :], scale=inv_n)
            S.recip(rstd, tmp)

        S.barrier()
        if stop == "C0":
            return nc
        conv_qs = []
        for l_ in range(DEPTH):
            q_ = []
            for e_ in range(NE):
                for part, (wsrc, cc) in enumerate(((w_gate, 8), (w_up, 8), (w_down, 4))):
                    q_.append((wsrc[l_, e_].rearrange("(c p) f -> p c f", p=128),
                               WB2[l_, e_ * 128:(e_ + 1) * 128, part * 4096:(part + 1) * 4096].rearrange("p (c f) -> p c f", c=cc),
                               ('WB', l_, e_, part)))
            conv_qs.append(q_)
        with ExitStack() as st:
            scraw = sb(st, "scraw", [128, 3, 8], F32)
            scT = sb(st, "scT", [128, 8, 32], F32)
            adab = sb(st, "adab", [32, 6 * D], F32)
            wsl = ring(st, "wsl", 4, [128, 8, 512], F32)
            mrow = ring(st, "mrow", 4, [32, 512], F32)
            pm = ring(st, "pm", 4, [128, 512], F32, psum=True)
            for r in range(3):
                S.ld(scraw[:, r, :], cvec[r].rearrange("(p c) -> p c", c=8))
            S.memset('dve', scT, 0.0)
            for r in range(3):
                S.act(scT[:, :, r], scraw[:, r, :], AF.Silu)
            for l in range(DEPTH):
                S.ld(adab, ada_b[l].partition_broadcast(32))
                awv = ada_w[l].rearrange("(p c) n -> p c n", c=8)
                for s in range(12):
                    w = wsl.next()
                    S.ld(w, awv[:, :, s * 512:(s + 1) * 512])
                    if conv_qs[0]:
                        src_, dst_, key_ = conv_qs[0].pop(0)
                        S.dma('pool', lambda en, src_=src_, dst_=dst_: en.dma_start(out=dst_, in_=src_), R=[], W=[key_])
                    p = pm.next()
                    for c in range(8):
                        S.mm(p[0:32, :], scT[:, c, :], w[:, c, :], start=(c == 0), stop=(c == 7))
                    m = mrow.next()
                    S.tt('dve', m, p[0:32, :], adab[:, s * 512:(s + 1) * 512], ALU.add)
                    S.st(MOD[l, :, s * 512:(s + 1) * 512], 'MOD', m[0:3, :])
        S.barrier()

        if stop == "L0":
            return nc

        def x_src(l, b, n):
            if l == 0:
                if n < 2:
                    return ctx_in[b, n * 128:(n + 1) * 128, :], None
                return x_in[b, (n - 2) * 128:(n - 1) * 128, :], None
            return XS[b, n * 128:(n + 1) * 128, :], ('XS', b, n)

        def load_mod_tiles(st, l, rows_segs, gvec, pfx):
            return None

        for l in range(DEPTH):
            last = (l == DEPTH - 1)
            WB = WB2[l]
            conv_q = conv_qs[l]
            conv_next = conv_qs[l + 1] if l + 1 < DEPTH else []

            def conv_issue(k, q=None):
                q = conv_q if q is None else q
                for _ in range(k):
                    if not q:
                        return
                    src_, dst_, key_ = q.pop(0)
                    S.dma('pool', lambda en, src_=src_, dst_=dst_: en.dma_start(out=dst_, in_=src_), R=[], W=[key_])

            with ExitStack() as lst:
                w_in_sb = sb(lst, "w_in_sb", [128, 8, 2560], BF16)
                w_out_sb = sb(lst, "w_out_sb", [128, 8, 1024], BF16)
                rope_c = sb(lst, "rope_c", [128, 16, 128], F32)
                rope_s = sb(lst, "rope_s", [128, 16, 128], F32)
                band = sb(lst, "band", [128, 20, 128], BF16)
                poolw = sb(lst, "poolw", [128, 4, 128], BF16)
                psc = sb(lst, "psc", [128, 4], F32)
                mtab = sb(lst, "mtab", [128, 4, 128], F32)
                maskT = sb(lst, "maskT", [128, 4, 128], F32)
                dtab = sb(lst, "dtab", [128, 6, 4], F32)
                lg = sb(lst, "lg", [128, 2, 4], F32)
                g1t = sb(lst, "g1t", [128, D], F32)
                qT = sb(lst, "qT", [128, 4, TT], BF16)
                kT = sb(lst, "kT", [128, 4, TT], BF16)

                winv = w_in[l].rearrange("(c p) n -> p c n", p=128)
                for c in range(8):
                    for j in range(5):
                        S.ld(w_in_sb[:, c, j * 512:(j + 1) * 512], winv[:, c, j * 512:(j + 1) * 512], q='pool')
                woutv = w_out[l].rearrange("(c p) n -> p c n", p=128)
                for c in range(8):
                    for j in range(2):
                        S.ld(w_out_sb[:, c, j * 512:(j + 1) * 512], woutv[:, c, j * 512:(j + 1) * 512], q='pool')
                S.ld(rope_c, c_rope_c[:, :, :])
                S.ld(rope_s, c_rope_s[:, :, :])
                S.ld(band, c_band[:, :, :], q='pool')
                S.ld(poolw, pool_w[l].rearrange("g c d -> c g d"), q='pool')
                for g in range(4):
                    S.ld(psc[:, g:g + 1], pool_scale[l, g * 128:(g + 1) * 128].rearrange("(p o) -> p o", o=1))
                S.ld(mtab, c_mtab[:, :, :])
                S.ld(g1t, norm1_g[l].partition_broadcast(128))
                S.ld(lg[:, 0, :], decay_f[l].partition_broadcast(128))
                S.ld(lg[:, 1, :], decay_b[l].partition_broadcast(128))
                S.act(lg, lg, AF.Exp, scale=-1.0)
                S.act(lg, lg, AF.Ln, bias=1.0)
                S.ts('dve', lg, lg, -1.0, ALU.mult)
                for ti, (dr, pc) in enumerate([(0, 0), (1, 1), (0, 2), (1, 3), (0, 4), (1, 4)]):
                    S.ts('dve', dtab[:, ti, :], lg[:, dr, :], pcols[:, pc:pc + 1], ALU.mult)
                S.act(dtab, dtab, AF.Exp)
                S.ts('dve', dtab[:, 0:2, :], dtab[:, 0:2, :], KS, ALU.mult)
                with ExitStack() as st:
                    e1 = sb(st, "e1", [128, 128], F32)
                    e2 = sb(st, "e2", [128, 128], F32)
                    for h in range(4):
                        S.act(e1, mtab[:, 0, :], AF.Exp, scale=lg[:, 0, h:h + 1])
                        S.tt('dve', e1, e1, mtab[:, 1, :], ALU.mult)
                        S.act(e2, mtab[:, 2, :], AF.Exp, scale=lg[:, 1, h:h + 1])
                        S.tt('dve', e2, e2, mtab[:, 3, :], ALU.mult)
                        S.tt('dve', e1, e1, e2, ALU.add)
                        S.ts('dve', maskT[:, h, :], e1, KS, ALU.mult)
                S.barrier()
                if stop == f"LS_{l}":
                    return nc

                for b in range(NB):
                    with ExitStack() as st:
                        Am = {}
                        Bm = {}
                        for r in (b, 2):
                            sc = sb(st, f"sc{r}", [128, D], F32)
                            Am[r] = sb(st, f"Am{r}", [128, D], F32)
                            Bm[r] = sb(st, f"Bm{r}", [128, D], F32)
                            S.ld(sc, MOD[l, r, 1 * D:2 * D].partition_broadcast(128), 'MOD')
                            S.ld(Bm[r], MOD[l, r, 0:D].partition_broadcast(128), 'MOD')
                            S.stt(Am[r], sc, 1.0, g1t, ALU.add, ALU.mult)
                        xt_r = ring(st, "xt", 2, [128, D], F32)
                        junk = sb(st, "junk", [128, D], BF16)
                        ssq_r = ring(st, "ssq", 2, [128, 1], F32)
                        tmp1_r = ring(st, "tmp1", 2, [128, 1], F32)
                        rstd_r = ring(st, "rstd", 2, [128, 1], F32)
                        t32_r = ring(st, "t32", 2, [128, D], F32)
                        hb_r = ring(st, "hb", 2, [128, D], BF16)
                        hT_r = ring(st, "hT", 2, [128, 8, 128], BF16)
                        r1 = sb(st, "r1", [128, 512], F32)
                        r2 = sb(st, "r2", [128, 512], F32)
                        qtm_r = ring(st, "qtm", 2, [128, 512], BF16)
                        stage_r = ring(st, "stage", 2, [128, 2048], BF16)
                        pT = ps(st, "pT", [128, 1024], BF16)
                        pP = [ps(st, f"pP{j}", [128, 512], F32) for j in range(5)]
                        pQK = ps(st, "pQK", [128, 1024], BF16)
                        cx = {}

                        def a0(n):
                            c_ = cx[n] = {}
                            conv_issue(2)
                            src, skey = x_src(l, b, n)
                            c_['xt'] = xt_r.next()
                            S.ld(c_['xt'], src, skey)

                        def a1(n):
                            c_ = cx[n]
                            r = 2 if n < 2 else b
                            xt = c_['xt']
                            ssq = ssq_r.next(); tmp1 = tmp1_r.next(); rstd = rstd_r.next()
                            t32 = t32_r.next()
                            c_['hb'] = hb_r.next()
                            S.act(junk, xt, AF.Square, accum=ssq)
                            rstd_from_ssq(rstd, ssq, tmp1, 1.0 / D)
                            S.stt(t32, xt, rstd, Am[r], ALU.mult, ALU.mult)
                            S.tt('pool', c_['hb'], t32, Bm[r], ALU.add)

                        def a2(n):
                            c_ = cx[n]
                            hb = c_['hb']
                            for c in range(8):
                                S.tr(pT[:, c * 128:(c + 1) * 128], hb[:, c * 128:(c + 1) * 128], ident_b)
                            c_['hT'] = hT_r.next()
                            S.cp('act', c_['hT'], pT.re("p (c t) -> p c t", c=8))

                        def a3(n):
                            c_ = cx[n]
                            hT = c_['hT']
                            for j in range(5):
                                for c in range(8):
                                    S.mm(pP[j], hT[:, c, :], w_in_sb[:, c, j * 512:(j + 1) * 512],
                                         start=(c == 0), stop=(c == 7))

                        def a4(n):
                            c_ = cx[n]
                            isctx = n < 2
                            stage = c_['stage'] = stage_r.next()
                            qtm = c_['qtm'] = qtm_r.next()
                            if isctx:
                                S.cp('act', qtm, pP[0])
                                S.cp('dve', stage[:, 0:512], pP[1])
                            else:
                                tn = n - 2
                                cb = rope_c[:, tn, :].bc(1, [128, 4, 128])
                                for (src_p, dst) in ((pP[0], qtm), (pP[1], stage[:, 0:512])):
                                    p3 = src_p.re("p (h d) -> p h d", h=4)
                                    p5 = src_p.re("p (h f a d) -> p h f a d", h=4, f=2, a=2)
                                    r25 = r2.re("p (h f a d) -> p h f a d", h=4, f=2, a=2)
                                    s5 = rope_s[:, tn, :].re("p (f a d) -> p f a d", f=2, a=2)
                                    S.tt('dve', r1.re("p (h d) -> p h d", h=4), p3, cb, ALU.mult)
                                    for a in range(2):
                                        S.tt('dve', r25[:, :, :, a, :], p5[:, :, :, 1 - a, :],
                                             s5[:, :, a, :].bc(1, [128, 4, 2, 32]), ALU.mult)
                                    S.tt('pool', dst, r1, r2, ALU.add)
                            S.cp('act', stage[:, 512:1024], pP[2])
                            S.act(stage[:, 1024:1536], pP[3], AF.Silu)
                            S.cp('act', stage[:, 1536:2048], pP[4])
                            S.st(GXKV[b, n * 128:(n + 1) * 128, :], ('GXKV', b, n), stage)

                        def a5(n):
                            c_ = cx[n]
                            qtm = c_['qtm']; stage = c_['stage']
                            for h in range(4):
                                S.tr(pQK[:, h * 128:(h + 1) * 128], qtm[:, h * 128:(h + 1) * 128], ident_b)
                            for h in range(4):
                                S.tr(pQK[:, 512 + h * 128:512 + (h + 1) * 128], stage[:, h * 128:(h + 1) * 128], ident_b)
                            S.cp('act', qT.sub((slice(None), slice(None), slice(n * 128, (n + 1) * 128)), ('qT', n)),
                                 pQK[:, 0:512].re("p (h t) -> p h t", h=4))
                            S.cp('dve', kT.sub((slice(None), slice(None), slice(n * 128, (n + 1) * 128)), ('kT', n)),
                                 pQK[:, 512:1024].re("p (h t) -> p h t", h=4))
                            del cx[n]

                        pipeline(NT, [(a0, 2), (a1, 1), (a2, 1), (a3, 0), (a4, 0), (a5, -1)])
                    S.barrier()
                    if stop == f"M1_{l}_{b}":
                        return nc

                    with ExitStack() as st:
                        S32 = sb(st, "S32", [128, 512], F32)
                        sbf_r = ring(st, "sbf", 2, [128, 512], BF16)
                        kv_r = ring(st, "kv", 3, [128, 1024], BF16)
                        vb_r = ring(st, "vb", 2, [128, 512], BF16)
                        pU_r = ring(st, "pU", 2, [128, 512], F32, psum=True)
                        S.memset('dve', S32, 0.0)
                        order = [1, 0] + list(range(NT - 1, 1, -1))
                        cx = {}

                        def b0(i):
                            n = order[i]
                            c_ = cx[i] = {}
                            c_['kv'] = kv_r.next()
                            S.ld(c_['kv'], GXKV[b, n * 128:(n + 1) * 128, 0:1024], ('GXKV', b, n))

                        def b1(i):
                            c_ = cx[i]
                            kv = c_['kv']
                            vb = vb_r.next()
                            S.tt('pool', vb.re("p (h e) -> p h e", h=4), kv[:, 512:1024].re("p (h e) -> p h e", h=4),
                                 dtab[:, 3, :].bc(2, [128, 4, 128]), ALU.mult)
                            pU = c_['pU'] = pU_r.next()
                            for h in range(4):
                                S.mm(pU[:, h * 128:(h + 1) * 128], kv[:, h * 128:(h + 1) * 128],
                                     vb[:, h * 128:(h + 1) * 128])

                        def b2(i):
                            n = order[i]
                            c_ = cx[i]
                            sbf = sbf_r.next()
                            S.cp('act', sbf, S32)
                            S.st(SBX[b, n, :, :], ('SBX', b, n), sbf)
                            S.tt('dve', S32.re("p (h e) -> p h e", h=4), S32.re("p (h e) -> p h e", h=4),
                                 dtab[:, 5, :].bc(2, [128, 4, 128]), ALU.mult)
                            S.tt('dve', S32, S32, c_['pU'], ALU.add)
                            del cx[i]

                        pipeline(NT, [(b0, 2), (b1, 1), (b2, 0)])
                    S.barrier()

                    with ExitStack() as st:
                        G1 = {}
                        for r in (b, 2):
                            G1[r] = sb(st, f"G1{r}", [128, D], F32)
                            S.ld(G1[r], MOD[l, r, 2 * D:3 * D].partition_broadcast(128), 'MOD')
                        S32 = sb(st, "S32f", [128, 512], F32)
                        sfb_r = ring(st, "sfb", 2, [128, 512], BF16)
                        gx_r = ring(st, "gx", 3, [128, 2048], BF16)
                        sbx_r = ring(st, "sbx", 3, [128, 512], BF16)
                        xpp_r = ring(st, "xpp", 2, [128, 512], BF16)
                        xpn_r = ring(st, "xpn", 2, [128, 512], BF16)
                        xt_r = ring(st, "xt3", 4, [128, D], F32)
                        pm_r = ring(st, "pmk", 2, [128, 512], BF16)
                        o32 = sb(st, "o32", [128, 512], F32)
                        u32 = sb(st, "u32", [128, 512], F32)
                        sq32 = sb(st, "sq32", [128, 512], F32)
                        ssqh = sb(st, "ssqh", [128, 4], F32)
                        tmph = sb(st, "tmph", [128, 4], F32)
                        rsh = sb(st, "rsh", [128, 4], F32)
                        ret_r = ring(st, "ret", 2, [128, 512], BF16)
                        mT_r = ring(st, "mT", 3, [128, 8, 128], BF16)
                        dT = sb(st, "dT", [128, 4, 128], BF16)
                        yg = sb(st, "yg", [128, D], F32)
                        xn_r = ring(st, "xn", 2, [128, D], F32)
                        vf_r = ring(st, "vf", 2, [128, 512], BF16)
                        pS = ps(st, "pS", [128, 512], F32)
                        pA = ps(st, "pA", [128, 512], F32)
                        pB = ps(st, "pB", [128, 512], F32)
                        pC = ps(st, "pC", [128, 512], F32)
                        pR = ps(st, "pR", [128, 1024], BF16)
                        pD = ps(st, "pD", [128, 512], F32)
                        pY = [ps(st, f"pY{j}", [128, 512], F32) for j in range(2)]
                        S.memset('dve', S32, 0.0)
                        sfb0 = sfb_r.next()
                        S.cp('act', sfb0, S32)
                        cx = {}
                        sfb_cur = {0: sfb0}

                        def c0(n):
                            c_ = cx[n] = {}
                            conv_issue(2)
                            isctx = n < 2
                            c_['do_out'] = not (last and isctx)
                            c_['first'] = n in (0, 2)
                            c_['lastt'] = n in (1, NT - 1)
                            gx = c_['gx'] = gx_r.next()
                            S.ld(gx, GXKV[b, n * 128:(n + 1) * 128, :], ('GXKV', b, n))
                            if c_['do_out']:
                                c_['sbx'] = sbx_r.next()
                                S.ld(c_['sbx'], SBX[b, n, :, :], ('SBX', b, n))
                                c_['xt'] = xt_r.next()
                                src, skey = x_src(l, b, n)
                                S.ld(c_['xt'], src, skey)
                                c_['xpp'] = c_['xpn'] = None
                                if not c_['first']:
                                    c_['xpp'] = xpp_r.next()
                                    S.ld(c_['xpp'], GXKV[b, (n - 1) * 128:n * 128, 1536:2048], ('GXKV', b, n - 1))
                                if not c_['lastt']:
                                    c_['xpn'] = xpn_r.next()
                                    S.ld(c_['xpn'], GXKV[b, (n + 1) * 128:(n + 2) * 128, 1536:2048], ('GXKV', b, n + 1))

                        def c1(n):
                            c_ = cx[n]
                            if not c_['do_out']:
                                return
                            gx = c_['gx']
                            qTn = qT.sub((slice(None), slice(None), slice(n * 128, (n + 1) * 128)), ('qT', n))
                            kTn = kT.sub((slice(None), slice(None), slice(n * 128, (n + 1) * 128)), ('kT', n))
                            for h in range(4):
                                S.mm(pS[:, h * 128:(h + 1) * 128], kTn[:, h, :], qTn[:, h, :])
                            pmk = c_['pmk'] = pm_r.next()
                            S.tt('dve', pmk, pS, maskT.re("p h i -> p (h i)"), ALU.mult)
                            mT = c_['mT'] = mT_r.next()
                            xpp = c_['xpp']; xpn = c_['xpn']
                            for g in range(4):
                                terms = []
                                if xpp is not None:
                                    terms.append((xpp[:, g * 128:(g + 1) * 128], band[:, g * 5 + 0, :]))
                                kind = 3 if c_['first'] else (4 if c_['lastt'] else 1)
                                terms.append((gx[:, 1536 + g * 128:1536 + (g + 1) * 128], band[:, g * 5 + kind, :]))
                                if xpn is not None:
                                    terms.append((xpn[:, g * 128:(g + 1) * 128], band[:, g * 5 + 2, :]))
                                for ti, (a_, b_) in enumerate(terms):
                                    S.mm(pD[:, g * 128:(g + 1) * 128], a_, b_, start=(ti == 0),
                                         stop=(ti == len(terms) - 1))
                            S.cp('act', dT, pD.re("p (g t) -> p g t", g=4))
                            for g in range(4):
                                S.mm(pD[:, g * 128:(g + 1) * 128], poolw[:, g, :], dT[:, g, :])
                            S.tt('dve', mT[:, 4:8, :], pD.re("p (g t) -> p g t", g=4),
                                 psc.bc(2, [128, 4, 128]), ALU.mult)

                        def c2(n):
                            c_ = cx[n]
                            gx = c_['gx']
                            ksl = gx[:, 0:512]
                            vsl = gx[:, 512:1024]
                            sfb = sfb_cur[n]
                            if c_['do_out']:
                                qTn = qT.sub((slice(None), slice(None), slice(n * 128, (n + 1) * 128)), ('qT', n))
                                pmk = c_['pmk']; sbx = c_['sbx']
                                for h in range(4):
                                    S.mm(pA[:, h * 128:(h + 1) * 128], pmk[:, h * 128:(h + 1) * 128],
                                         vsl[:, h * 128:(h + 1) * 128])
                                for h in range(4):
                                    S.mm(pB[:, h * 128:(h + 1) * 128], qTn[:, h, :], sfb[:, h * 128:(h + 1) * 128])
                                for h in range(4):
                                    S.mm(pC[:, h * 128:(h + 1) * 128], qTn[:, h, :], sbx[:, h * 128:(h + 1) * 128])
                            vf = vf_r.next()
                            S.tt('pool', vf.re("p (h e) -> p h e", h=4), vsl.re("p (h e) -> p h e", h=4),
                                 dtab[:, 2, :].bc(2, [128, 4, 128]), ALU.mult)
                            for h in range(4):
                                S.mm(pS[:, h * 128:(h + 1) * 128], ksl[:, h * 128:(h + 1) * 128],
                                     vf[:, h * 128:(h + 1) * 128])
                            S.tt('dve', S32.re("p (h e) -> p h e", h=4), S32.re("p (h e) -> p h e", h=4),
                                 dtab[:, 4, :].bc(2, [128, 4, 128]), ALU.mult)
                            S.tt('dve', S32, S32, pS, ALU.add)
                            sfbn = sfb_r.next()
                            S.cp('act', sfbn, S32)
                            sfb_cur[n + 1] = sfbn
                            if c_['do_out']:
                                S.tt('dve', o32.re("p (h e) -> p h e", h=4), pB.re("p (h e) -> p h e", h=4),
                                     dtab[:, 0, :].bc(2, [128, 4, 128]), ALU.mult)
                                S.tt('dve', u32.re("p (h e) -> p h e", h=4), pC.re("p (h e) -> p h e", h=4),
                                     dtab[:, 1, :].bc(2, [128, 4, 128]), ALU.mult)
                                S.tt('pool', o32, o32, u32, ALU.add)
                                S.tt('dve', o32, o32, pA, ALU.add)
                                S.act(sq32, o32, AF.Square)
                                S.red(ssqh, sq32.re("p (h e) -> p h e", h=4), ALU.add)
                                rstd_from_ssq(rsh, ssqh, tmph, 1.0 / 128)
                                S.tt('pool', u32.re("p (h e) -> p h e", h=4), o32.re("p (h e) -> p h e", h=4),
                                     rsh.bc(2, [128, 4, 128]), ALU.mult)
                                ret = c_['ret'] = ret_r.next()
                                S.tt('pool', ret, u32, gx[:, 1024:1536], ALU.mult)

                        def c3(n):
                            c_ = cx[n]
                            if c_['do_out']:
                                r = 2 if n < 2 else b
                                ret = c_['ret']; mT = c_['mT']; xt = c_['xt']
                                for h in range(4):
                                    S.tr(pR[:, h * 128:(h + 1) * 128], ret[:, h * 128:(h + 1) * 128], ident_b)
                                S.cp('act', mT[:, 0:4, :], pR[:, 0:512].re("p (h t) -> p h t", h=4))
                                for j in range(2):
                                    for c in range(8):
                                        S.mm(pY[j], mT[:, c, :], w_out_sb[:, c, j * 512:(j + 1) * 512],
                                             start=(c == 0), stop=(c == 7))
                                for j in range(2):
                                    S.tt('dve', yg[:, j * 512:(j + 1) * 512], pY[j], G1[r][:, j * 512:(j + 1) * 512],
                                         ALU.mult)
                                xn = xn_r.next()
                                S.tt('pool', xn, xt, yg, ALU.add)
                                S.st(XS[b, n * 128:(n + 1) * 128, :], ('XS', b, n), xn)
                            del cx[n]

                        pipeline(NT, [(c0, 2), (c1, 1), (c2, 0), (c3, -1)])
                    S.barrier()
                    if stop == f"M3_{l}_{b}":
                        return nc
            S.barrier()
            if stop == f"MIX_{l}":
                return nc

            conv_issue(len(conv_q))
            S.barrier()
            tiles = [(b, n) for b in range(NB) for n in range(NT) if not (last and n < 2)]
            NG = len(tiles)
            with ExitStack() as lst:
                OH1 = sb(lst, "OH1", [128, NG, 32], F32)
                OH2 = sb(lst, "OH2", [128, NG, 32], F32)
                W12 = sb(lst, "W12", [128, NG, 2], F32)
                RK = sb(lst, "RK", [128, NG, 2], F32)
                DEST = sb(lst, "DEST", [128, NG, 2], I32)
                widx = sb(lst, "widx", [128, NBLK], I32)
                g2t = sb(lst, "g2t", [128, D], F32)
                pstart = sb(lst, "pstart", [128, 32], F32)
                S.ld(g2t, norm2_g[l].partition_broadcast(128))
                rows = sorted(set(2 if n < 2 else b for (b, n) in tiles))
                with ExitStack() as st:
                    Am = {}
                    Bm = {}
                    for r in rows:
                        sc = sb(st, f"sc2{r}", [128, D], F32)
                        Am[r] = sb(st, f"A2{r}", [128, D], F32)
                        Bm[r] = sb(st, f"B2{r}", [128, D], F32)
                        S.ld(sc, MOD[l, r, 4 * D:5 * D].partition_broadcast(128), 'MOD')
                        S.ld(Bm[r], MOD[l, r, 3 * D:4 * D].partition_broadcast(128), 'MOD')
                        S.stt(Am[r], sc, 1.0, g2t, ALU.add, ALU.mult)
                    wr = sb(st, "wr", [128, 8, 36], F32)
                    rbias = sb(st, "rbias", [128, 36], F32)
                    utri = sb(st, "utri", [128, 128], F32)
                    Racc = sb(st, "Racc", [128, 32], F32)
                    zt = sb(st, "zt", [128, 4096], BF16)
                    S.memset('pool', zt, 0.0)
                    HBv = HB.rearrange("(p a) d -> p (a d)", p=128)
                    for kz in range(NBLK * D // 4096):
                        S.st(HBv[:, kz * 4096:(kz + 1) * 4096], 'HB', zt)
                    S.ld(wr, router_w[l].rearrange("(c p) n -> p c n", p=128))
                    S.ld(rbias, router_b[l].partition_broadcast(128))
                    S.ld(utri, c_utri[:, :])
                    S.memset('dve', Racc, 0.0)
                    xt_r = ring(st, "xtr", 2, [128, D], F32)
                    junk = sb(st, "junkr", [128, D], BF16)
                    ssq = sb(st, "ssqr", [128, 1], F32)
                    tmp1 = sb(st, "tmp1r", [128, 1], F32)
                    rstd = sb(st, "rstdr", [128, 1], F32)
                    t32 = sb(st, "t32r", [128, D], F32)
                    h32_r = ring(st, "h32", 2, [128, D], F32)
                    hbp_r = ring(st, "hbp", 2, [128, D], BF16)
                    h32T = sb(st, "h32T", [128, 8, 128], F32)
                    lgt = sb(st, "lgt", [128, 36], F32)
                    sm = sb(st, "sm", [128, 16], F32)
                    ohg = sb(st, "ohg", [128, 4], F32)
                    j4 = sb(st, "j4", [128, 4], F32)
                    sel = sb(st, "sel", [128, 4, 8], F32)
                    ein = sb(st, "ein", [128, 8], F32)
                    m8 = sb(st, "m8", [128, 8], F32)
                    eq1 = sb(st, "eq1", [128, 8], F32)
                    eq2 = sb(st, "eq2", [128, 8], F32)
                    OHs = sb(st, "OHs", [128, 32], F32)
                    t32b = sb(st, "t32b", [128, 32], F32)
                    pTr = [ps(st, f"pTr{j}", [128, 512], F32) for j in range(2)]
                    pL = ps(st, "pL", [128, 512], F32)
                    pCn = ps(st, "pCn", [128, 512], F32)
                    cx = {}

                    def r0(gt):
                        b, n = tiles[gt]
                        c_ = cx[gt] = {}
                        conv_issue(2, conv_next)
                        c_['xt'] = xt_r.next()
                        S.ld(c_['xt'], XS[b, n * 128:(n + 1) * 128, :], ('XS', b, n))

                    def r1(gt):
                        b, n = tiles[gt]
                        c_ = cx[gt]
                        r = 2 if n < 2 else b
                        xt = c_['xt']
                        S.act(junk, xt, AF.Square, accum=ssq)
                        S.act(tmp1, ssq, AF.Ln, bias=epsc, scale=1.0 / D)
                        S.act(rstd, tmp1, AF.Exp, scale=-0.5)
                        S.stt(t32, xt, rstd, Am[r], ALU.mult, ALU.mult)
                        h32 = c_['h32'] = h32_r.next()
                        S.tt('pool', h32, t32, Bm[r], ALU.add)
                        hbp = hbp_r.next()
                        S.cp('act', hbp, h32)
                        S.st(HT[gt * 128:(gt + 1) * 128, :], ('HT', gt), hbp)

                    def r2(gt):
                        c_ = cx[gt]
                        h32 = c_['h32']
                        for c in range(8):
                            S.tr(pTr[c // 4][:, (c % 4) * 128:(c % 4 + 1) * 128], h32[:, c * 128:(c + 1) * 128], ident_f)
                        S.cp('dve', h32T[:, 0:4, :], pTr[0].re("p (c t) -> p c t", c=4))
                        S.cp('act', h32T[:, 4:8, :], pTr[1].re("p (c t) -> p c t", c=4))
                        for c in range(8):
                            S.mm(pL[:, 0:36], h32T[:, c, :], wr[:, c, :], start=(c == 0), stop=(c == 7))
                        S.tt('dve', lgt, pL[:, 0:36], rbias, ALU.add)
                        gl = lgt[:, 0:4]
                        S.red(sm[:, 0:1], gl, ALU.max)
                        S.ts('dve', ohg, gl, sm[:, 0:1], ALU.is_equal)
                        S.ts('dve', sm[:, 1:2], sm[:, 0:1], -1.0, ALU.mult)
                        S.act(j4, gl, AF.Exp, bias=sm[:, 1:2], accum=sm[:, 2:3])
                        S.recip(sm[:, 3:4], sm[:, 2:3])
                        S.tt('dve', sel, lgt[:, 4:36].re("p (g e) -> p g e", g=4), ohg.bc(2, [128, 4, 8]), ALU.mult)
                        S.red(ein, sel.re("p g e -> p e g"), ALU.add)
                        S.op('dve', lambda e: e.max(out=m8.ap, in_=ein.ap), R=[ein.key], W=[m8.key])
                        S.ts('dve', eq1, ein, m8[:, 0:1], ALU.is_equal)
                        S.ts('dve', eq2, ein, m8[:, 1:2], ALU.is_equal)
                        S.tt('dve', sm[:, 4:5], m8[:, 1:2], m8[:, 0:1], ALU.subtract)
                        S.act(sm[:, 5:6], sm[:, 4:5], AF.Exp)
                        S.ts('dve', sm[:, 6:7], sm[:, 5:6], 1.0, ALU.add)
                        S.recip(sm[:, 7:8], sm[:, 6:7])
                        w12 = W12.sub((slice(None), gt, slice(None)), ('W12', gt))
                        S.tt('dve', w12[:, 0:1], sm[:, 7:8], sm[:, 3:4], ALU.mult)
                        S.tt('dve', w12[:, 1:2], w12[:, 0:1], sm[:, 5:6], ALU.mult)
                        oh1 = OH1.sub((slice(None), gt, slice(None)), ('OH1', gt))
                        oh2 = OH2.sub((slice(None), gt, slice(None)), ('OH2', gt))
                        S.tt('dve', oh1.re("p (g e) -> p g e", g=4), ohg.bc(2, [128, 4, 8]), eq1.bc(1, [128, 4, 8]), ALU.mult)
                        S.tt('dve', oh2.re("p (g e) -> p g e", g=4), ohg.bc(2, [128, 4, 8]), eq2.bc(1, [128, 4, 8]), ALU.mult)
                        S.tt('dve', OHs, oh1, oh2, ALU.add)
                        S.mm(pCn[:, 0:32], utri, OHs, start=True, stop=False)
                        S.mm(pCn[:, 0:32], ones_f, Racc, start=False, stop=True)
                        rk = RK.sub((slice(None), gt, slice(None)), ('RK', gt))
                        S.tt('dve', t32b, oh1, pCn[:, 0:32], ALU.mult)
                        S.red(rk[:, 0:1], t32b, ALU.add)
                        S.tt('dve', t32b, oh2, pCn[:, 0:32], ALU.mult)
                        S.red(rk[:, 1:2], t32b, ALU.add)
                        S.tt('dve', Racc, Racc, OHs, ALU.add)
                        del cx[gt]

                    pipeline(NG, [(r0, 2), (r1, 1), (r2, 0)])
                    cnt = sb(st, "cnt", [128, 32], F32)
                    cnti = sb(st, "cnti", [128, 32], I32)
                    padded = sb(st, "padded", [128, 32], F32)
                    ca = sb(st, "ca", [128, 32], F32)
                    cb_ = sb(st, "cb_", [128, 32], F32)
                    cmp = sb(st, "cmp", [128, NBLK, 32], F32)
                    bef = sb(st, "bef", [128, NBLK], F32)
                    same = sb(st, "same", [128, NBLK], F32)
                    blk = sb(st, "blk", [128, NBLK], F32)
                    S.ld(blk, c_blk[:, :])
                    S.mm(pCn[:, 0:32], ones_f, Racc)
                    S.cp('dve', cnt, pCn[:, 0:32])
                    S.ts('dve', cnti, cnt, 127.0, ALU.add)
                    S.ts('dve', cnti, cnti, 7, ALU.arith_shift_right, s2=7, op1=ALU.logical_shift_left)
                    S.cp('dve', padded, cnti)
                    cur, oth = ca, cb_
                    S.cp('dve', cur, padded)
                    for s in (1, 2, 4, 8, 16):
                        S.cp('dve', oth, cur)
                        S.tt('dve', oth[:, s:32], cur[:, s:32], cur[:, 0:32 - s], ALU.add)
                        cur, oth = oth, cur
                    pends = cur
                    S.tt('dve', pstart, pends, padded, ALU.subtract)
                    S.tt('dve', cmp, pends.bc(1, [128, NBLK, 32]), blk.bc(2, [128, NBLK, 32]), ALU.is_le)
                    S.red(bef, cmp, ALU.add)
                    S.ts('dve', bef, bef, 31.0, ALU.min)
                    S.ts('dve', bef, bef, 128.0, ALU.mult, s2=pcols[:, 3:4], op1=ALU.add)
                    S.memset('dve', same, 0.0)
                    S.tt('dve', same[:, 1:NBLK], bef[:, 1:NBLK], bef[:, 0:NBLK - 1], ALU.is_equal)
                    for qq in range(1, NSTREAM):
                        S.memset('dve', same[:, qq * BPS:qq * BPS + 1], 0.0)
                    S.ts('dve', bef, bef, float(l * NE * 128), ALU.add)
                    S.stt(bef, same, float(BIGIDX), bef, ALU.mult, ALU.add)
                    S.cp('dve', widx, bef)
                    hb2_r = ring(st, "hb2", 2, [128, D], BF16)
                    dsf = sb(st, "dsf", [128, 2], F32)
                    for gt, (b, n) in enumerate(tiles):
                        rk = RK.sub((slice(None), gt, slice(None)), ('RK', gt))
                        dst = DEST.sub((slice(None), gt, slice(None)), ('DEST', gt))
                        for k, OHk in enumerate((OH1, OH2)):
                            ohk = OHk.sub((slice(None), gt, slice(None)), (OHk.key, gt))
                            S.tt('dve', t32b, ohk, pstart, ALU.mult)
                            S.red(dsf[:, k:k + 1], t32b, ALU.add)
                        S.tt('dve', dsf, dsf, rk, ALU.add)
                        S.cp('dve', dst, dsf)
                        hb2 = hb2_r.next()
                        S.ld(hb2, HT[gt * 128:(gt + 1) * 128, :], ('HT', gt))
                        for k in range(2):
                            S.dma('pool', lambda e, k=k, dst=dst, hb2=hb2: e.indirect_dma_start(
                                out=HB[:, :], out_offset=bass.IndirectOffsetOnAxis(ap=dst.ap[:, k:k + 1], axis=0),
                                in_=hb2.ap, in_offset=None), R=[hb2.key, dst.key], W=['HB'])
                S.barrier()
                if stop == f"R_{l}":
                    return nc

                with ExitStack() as st:
                    wb_r = ring(st, "wbuf", NSTREAM, [128, 12288], BF16)
                    hg_r = ring(st, "hg", 5, [128, D], BF16)
                    hgT_r = ring(st, "hgT", 2, [128, 8, 128], BF16)
                    sg_r = ring(st, "sg", 2, [128, 512], F32)
                    actb_r = ring(st, "actb", 2, [128, 512], BF16)
                    actT_r = ring(st, "actT", 2, [128, 4, 128], BF16)
                    yb_r = ring(st, "yb", 2, [128, D], F32)
                    pT = ps(st, "pTe", [128, 1024], BF16)
                    pG_r = ring(st, "pG", 2, [128, 512], F32, psum=True)
                    pUp_r = ring(st, "pUp", 2, [128, 512], F32, psum=True)
                    pT2 = ps(st, "pT2", [128, 1024], BF16)
                    pY = [ps(st, f"pYe{j}", [128, 512], F32) for j in range(2)]
                    cx = {}

                    def e_s0(j):
                        i = (j % NSTREAM) * BPS + j // NSTREAM
                        c_ = cx[j] = {'i': i}
                        wb_ = wb_r.next()
                        S.dma('pool', lambda e, wb_=wb_, i=i: e.indirect_dma_start(
                            out=wb_.ap, out_offset=None, in_=WB2.rearrange("l r x -> (l r) x"),
                            in_offset=bass.IndirectOffsetOnAxis(ap=widx.ap[:, i:i + 1], axis=0),
                            bounds_check=bc_reg, oob_is_err=False),
                            R=[widx.key], W=[wb_.key])
                        c_['wg3'] = wb_[:, 0:4096].re("p (c f) -> p c f", c=8)
                        c_['wu3'] = wb_[:, 4096:8192].re("p (c f) -> p c f", c=8)
                        c_['wd'] = wb_[:, 8192:12288].re("p (c f) -> p c f", c=4)
                        c_['hg'] = hg_r.next()
                        S.ld(c_['hg'], HB[i * 128:(i + 1) * 128, :], 'HB')

                    def e_s1(j):
                        c_ = cx[j]
                        hg = c_['hg']
                        for c in range(8):
                            S.tr(pT[:, c * 128:(c + 1) * 128], hg[:, c * 128:(c + 1) * 128], ident_b)
                        c_['hgT'] = hgT_r.next()
                        S.cp('act', c_['hgT'], pT.re("p (c t) -> p c t", c=8))

                    def e_s2(j):
                        c_ = cx[j]
                        c_['pG'] = pG_r.next()
                        c_['pUp'] = pUp_r.next()
                        for c in range(8):
                            S.mm(c_['pG'], c_['hgT'][:, c, :], c_['wg3'][:, c, :], start=(c == 0), stop=(c == 7))
                        for c in range(8):
                            S.mm(c_['pUp'], c_['hgT'][:, c, :], c_['wu3'][:, c, :], start=(c == 0), stop=(c == 7))

                    def e_s3(j):
                        c_ = cx[j]
                        sg = sg_r.next()
                        c_['actb'] = actb_r.next()
                        S.act(sg, c_['pG'], AF.Silu)
                        S.tt('dve', c_['actb'], sg, c_['pUp'], ALU.mult)

                    def e_s4(j):
                        c_ = cx[j]
                        for c in range(4):
                            S.tr(pT2[:, c * 128:(c + 1) * 128], c_['actb'][:, c * 128:(c + 1) * 128], ident_b)
                        c_['actT'] = actT_r.next()
                        S.cp('act', c_['actT'], pT2[:, 0:512].re("p (c t) -> p c t", c=4))

                    def e_s5(j):
                        c_ = cx[j]
                        for jj in range(2):
                            for c in range(4):
                                S.mm(pY[jj], c_['actT'][:, c, :], c_['wd'][:, c, jj * 512:(jj + 1) * 512],
                                     start=(c == 0), stop=(c == 3))
                        yb = yb_r.next()
                        S.cp('dve', yb[:, 0:512], pY[0])
                        S.cp('act', yb[:, 512:1024], pY[1])
                        i = c_['i']
                        S.st(YB[i * 128:(i + 1) * 128, :], 'YB', yb)
                        del cx[j]

                    pipeline(NBLK, [(e_s0, 4), (e_s1, 1), (e_s2, 0), (e_s3, 0), (e_s4, -1), (e_s5, -1)])
                S.barrier()
                if stop == f"E_{l}":
                    return nc

                with ExitStack() as st:
                    G2 = {}
                    for r in rows:
                        G2[r] = sb(st, f"G2{r}", [128, D], F32)
                        S.ld(G2[r], MOD[l, r, 5 * D:6 * D].partition_broadcast(128), 'MOD')
                    fg = sb(st, "fg", [128, D], F32)
                    S.ld(fg, fin_g.partition_broadcast(128))
                    y1_r = ring(st, "y1", 3, [128, D], F32)
                    y2_r = ring(st, "y2", 3, [128, D], F32)
                    xt_r = ring(st, "xtf", 3, [128, D], F32)
                    ya = sb(st, "ya", [128, D], F32)
                    xn_r = ring(st, "xnf", 2, [128, D], F32)
                    junk = sb(st, "junkf", [128, D], BF16)
                    ssq = sb(st, "ssqf", [128, 1], F32)
                    tmp1 = sb(st, "tmp1f", [128, 1], F32)
                    rstd = sb(st, "rstdf", [128, 1], F32)
                    ot_r = ring(st, "ot", 2, [128, D], F32)
                    cx = {}

                    def f0(gt):
                        b, n = tiles[gt]
                        c_ = cx[gt] = {}
                        dst = DEST.sub((slice(None), gt, slice(None)), ('DEST', gt))
                        c_['y1'] = y1_r.next()
                        c_['y2'] = y2_r.next()
                        for k, yk in enumerate((c_['y1'], c_['y2'])):
                            S.dma('pool', lambda e, k=k, yk=yk, dst=dst: e.indirect_dma_start(
                                out=yk.ap, out_offset=None, in_=YB[:, :],
                                in_offset=bass.IndirectOffsetOnAxis(ap=dst.ap[:, k:k + 1], axis=0)),
                                R=['YB', dst.key], W=[yk.key])
                        c_['xt'] = xt_r.next()
                        S.ld(c_['xt'], XS[b, n * 128:(n + 1) * 128, :], ('XS', b, n))

                    def f1(gt):
                        b, n = tiles[gt]
                        c_ = cx[gt]
                        r = 2 if n < 2 else b
                        w12 = W12.sub((slice(None), gt, slice(None)), ('W12', gt))
                        y1 = c_['y1']; y2 = c_['y2']; xt = c_['xt']
                        S.act(ya, y1, AF.Copy, scale=w12[:, 0:1])
                        S.stt(ya, y2, w12[:, 1:2], ya, ALU.mult, ALU.add)
                        S.tt('dve', ya, ya, G2[r], ALU.mult)
                        xn = xn_r.next()
                        S.tt('dve', xn, xt, ya, ALU.add)
                        if last:
                            S.act(junk, xn, AF.Square, accum=ssq)
                            rstd_from_ssq(rstd, ssq, tmp1, 1.0 / D)
                            ot = ot_r.next()
                            S.stt(ot, xn, rstd, fg, ALU.mult, ALU.mult)
                            S.st(out[b, (n - 2) * 128:(n - 1) * 128, :], ('out', b, n), ot)
                        else:
                            S.st(XS[b, n * 128:(n + 1) * 128, :], ('XS', b, n), xn)
                        del cx[gt]

                    pipeline(NG, [(f0, 2), (f1, 0)])
                S.barrier()
        S.barrier(engines=['sp'])
    return nc


def _consts():
    c = {}
    t = np.arange(T)
    rows = (t // 64).astype(np.float32)
    cols = (t % 64).astype(np.float32)
    inv = (10000.0 ** (-np.arange(32, dtype=np.float32) / 32)).astype(np.float32)
    ar = rows[:, None] * inv[None, :]
    ac = cols[:, None] * inv[None, :]
    C = np.concatenate([np.cos(ar), np.cos(ar), np.cos(ac), np.cos(ac)], axis=1)
    Sg = np.concatenate([-np.sin(ar), np.sin(ar), -np.sin(ac), np.sin(ac)], axis=1)
    c["c_rope_c"] = np.ascontiguousarray(C.reshape(16, 128, 128).transpose(1, 0, 2)).astype(np.float32)
    c["c_rope_s"] = np.ascontiguousarray(Sg.reshape(16, 128, 128).transpose(1, 0, 2)).astype(np.float32)
    band = np.zeros((128, 20, 128), np.float32)
    tp = np.arange(128)[:, None]
    tt = np.arange(128)[None, :]
    for g, w in enumerate((2, 4, 8, 16)):
        h = w // 2
        eye = (tp == tt).astype(np.float32)
        band[:, g * 5 + 0, :] = ((tp - 128) >= (tt - h)).astype(np.float32) / w
        band[:, g * 5 + 2, :] = ((tp + 128) <= (tt + h - 1)).astype(np.float32) / w
        inwin = ((tp >= tt - h) & (tp <= tt + h - 1)).astype(np.float32)
        band[:, g * 5 + 1, :] = inwin / w - eye
        cnt_first = (np.minimum(tt + h, 10 ** 9) - np.maximum(tt - h, 0)).astype(np.float32)
        band[:, g * 5 + 3, :] = inwin / cnt_first - eye
        cnt_last = (np.minimum(tt + h, 128) - (tt - h)).astype(np.float32)
        band[:, g * 5 + 4, :] = inwin / cnt_last - eye
    c["c_band"] = band
    c["c_ident"] = np.eye(128, dtype=np.float32)
    c["c_utri"] = (tp < tt).astype(np.float32)
    j = np.arange(128)[:, None].astype(np.float32)
    i = np.arange(128)[None, :].astype(np.float32)
    mt = np.zeros((128, 4, 128), np.float32)
    mt[:, 0, :] = np.maximum(i - j, 0)
    mt[:, 1, :] = (i >= j)
    mt[:, 2, :] = np.maximum(j - i, 0)
    mt[:, 3, :] = (j >= i)
    c["c_mtab"] = mt
    p = np.arange(128, dtype=np.float32)
    pc = np.zeros((128, 8), np.float32)
    pc[:, 0] = p + 1
    pc[:, 1] = 128 - p
    pc[:, 2] = 127 - p
    pc[:, 3] = p
    pc[:, 4] = 128
    c["c_pcols"] = pc
    c["c_blk"] = np.tile((np.arange(NBLK, dtype=np.float32) * 128)[None, :], (128, 1))
    return c


_NC_CACHE = {}


def _prep_inputs(inputs):
    f = lambda a: np.ascontiguousarray(np.asarray(a, dtype=np.float32))
    x = f(inputs["x"])
    ctx = f(inputs["ctx"])
    c = f(inputs["c"])
    c_ctx = f(inputs["c_ctx"])
    shared = {k: f(inputs[k]) for k in ("ada_w", "ada_b", "norm1_g", "w_in", "decay_fwd", "decay_bwd", "pool_w",
                                        "pool_scale", "w_out", "norm2_g", "exp_w_gate", "exp_w_up", "exp_w_down",
                                        "final_norm_g")}
    shared["router_w"] = np.ascontiguousarray(np.concatenate([f(inputs["router_g_w"]), f(inputs["router_e_w"])], axis=2))
    shared["router_b"] = np.ascontiguousarray(np.concatenate([f(inputs["router_g_b"]), f(inputs["router_e_b"])], axis=1))
    shared.update(_consts())
    in_maps = []
    for i in range(8):
        m = dict(shared)
        m["x"] = np.ascontiguousarray(x[2 * i:2 * i + 2])
        m["ctx"] = np.ascontiguousarray(ctx[2 * i:2 * i + 2])
        m["cvec"] = np.ascontiguousarray(np.stack([c[2 * i], c[2 * i + 1], c_ctx], axis=0))
        in_maps.append(m)
    return in_maps


def kernel(**inputs):
    in_maps = _prep_inputs(inputs)
    if "nc" not in _NC_CACHE:
        _NC_CACHE["nc"] = build()
    nc = _NC_CACHE["nc"]
    res = run_bass_kernel_spmd(nc, in_maps, core_ids=list(range(8)))
    return np.concatenate([np.asarray(r["out"], dtype=np.float32) for r in res.results], axis=0)
```

```python
import os
import numpy as np
from contextlib import ExitStack
import concourse.bass as bass
import concourse.mybir as mybir
from concourse.bass_utils import run_bass_kernel_spmd

F32 = mybir.dt.float32
BF16 = mybir.dt.bfloat16
I32 = mybir.dt.int32
AF = mybir.ActivationFunctionType
ALU = mybir.AluOpType
AX = mybir.AxisListType

D = 1024
T = 2048
NCX = 256
TT = T + NCX
NT = TT // 128
NB = 2
DEPTH = 2
NE = 32
NBLK = 108
NROWS = NBLK * 128
NSTREAM = 6
BPS = NBLK // NSTREAM
BIGIDX = 1 << 20
EPS = 1e-6
KS = 128.0 ** -0.5


class V:
    __slots__ = ("ap", "key")

    def __init__(self, ap, key):
        self.ap = ap
        self.key = key

    def __getitem__(self, idx):
        return V(self.ap[idx], self.key)

    def sub(self, idx, key):
        return V(self.ap[idx], key)

    def re(self, pat, **kw):
        return V(self.ap.rearrange(pat, **kw), self.key)

    def bc(self, axis, shape):
        return V(self.ap.unsqueeze(axis).to_broadcast(list(shape)), self.key)


class Sched:
    def __init__(self, nc, stack, n_dma_sems=8):
        self.nc = nc
        self.eng = {'pe': nc.tensor, 'dve': nc.vector, 'act': nc.scalar, 'pool': nc.gpsimd, 'sp': nc.sync}
        self.sem = {}
        self.cnt = {}
        self.waited = {e: {} for e in self.eng}
        for e in self.eng:
            self.sem[e] = stack.enter_context(nc.semaphore(f"s_{e}"))
            self.cnt[e] = 0
        self.dsem = {}
        self.dval = {}
        self.dnext = {}
        for q, nq in (('sp', 12), ('pool', 24)):
            self.dsem[q] = [stack.enter_context(nc.semaphore(f"d_{q}{i}")) for i in range(nq)]
            self.dval[q] = [0] * nq
            self.dnext[q] = 0
        self.last_w = {}
        self.readers = {}
        self.n_ins = 0
        self.excl = set()

    def _deps(self, reads, writes):
        deps = []
        for k in reads:
            if k in self.last_w:
                deps.append(self.last_w[k])
        for k in writes:
            if k in self.last_w:
                deps.append(self.last_w[k])
            deps.extend(self.readers.get(k, ()))
        return deps

    def _wait(self, e, deps):
        w = self.waited[e]
        need = {}
        for (sid, sem, val) in deps:
            if e == 'pe' and sid == 'c_pe':
                continue
            if w.get(sid, 0) >= val:
                continue
            if sid not in need or need[sid][1] < val:
                need[sid] = (sem, val)
        for sid, (sem, val) in need.items():
            self.eng[e].wait_ge(sem, val)
            w[sid] = val
            self.n_ins += 1

    def _commit(self, tok, reads, writes):
        for k in reads:
            self.readers.setdefault(k, []).append(tok)
        for k in writes:
            self.last_w[k] = tok
            self.readers[k] = []

    def op(self, e, fn, R=(), W=()):
        W = [k for k in W if k is not None] + [k for k in R if k in self.excl]
        R = [k for k in R if k is not None and k not in self.excl]
        self._wait(e, self._deps(R, W))
        ins = fn(self.eng[e])
        self.cnt[e] += 1
        ins.then_inc(self.sem[e], 1)
        tok = ('c_' + e, self.sem[e], self.cnt[e])
        self._commit(tok, R, W)
        self.n_ins += 1
        return tok

    def dma(self, q, fn, R=(), W=()):
        R = [k for k in R if k is not None]
        W = [k for k in W if k is not None]
        deps = self._deps(R, W)
        i = self.dnext[q]
        self.dnext[q] = (i + 1) % len(self.dsem[q])
        sem = self.dsem[q][i]
        sid = f'd_{q}{i}'
        if self.dval[q][i] > 0:
            deps.append((sid, sem, self.dval[q][i]))
        self._wait(q, deps)
        ins = fn(self.eng[q])
        self.dval[q][i] += 16
        ins.then_inc(sem, 16)
        tok = (sid, sem, self.dval[q][i])
        self._commit(tok, R, W)
        self.n_ins += 1
        return tok

    def all_tokens(self):
        toks = []
        for e in self.eng:
            if self.cnt[e] > 0:
                toks.append(('c_' + e, self.sem[e], self.cnt[e]))
        for q in self.dsem:
            for i, sem in enumerate(self.dsem[q]):
                if self.dval[q][i] > 0:
                    toks.append((f'd_{q}{i}', sem, self.dval[q][i]))
        return toks

    def barrier(self, engines=None):
        toks = self.all_tokens()
        for e in (engines or self.eng):
            self._wait(e, [t for t in toks if not (t[0] == 'c_' + e)])
        self.last_w = {}
        self.readers = {}

    def mm(self, out, lhsT, rhs, start=True, stop=True):
        return self.op('pe', lambda e: e.matmul(out.ap, lhsT=lhsT.ap, rhs=rhs.ap, start=start, stop=stop),
                       R=[lhsT.key, rhs.key], W=[out.key])

    def tr(self, out, in_, ident):
        return self.op('pe', lambda e: e.transpose(out.ap, in_.ap, ident.ap), R=[in_.key, ident.key], W=[out.key])

    def act(self, out, in_, func, bias=None, scale=None, accum=None, eng='act'):
        kw = {}
        R = [in_.key]
        W = [out.key]
        if bias is not None:
            if isinstance(bias, V):
                kw['bias'] = bias.ap
                R.append(bias.key)
            else:
                kw['bias'] = bias
        if scale is not None:
            if isinstance(scale, V):
                kw['scale'] = scale.ap
                R.append(scale.key)
            else:
                kw['scale'] = scale
        if accum is not None:
            kw['accum_out'] = accum.ap
            W.append(accum.key)
        return self.op('act', lambda e: e.activation(out=out.ap, in_=in_.ap, func=func, **kw), R=R, W=W)

    def tt(self, eng, out, in0, in1, op):
        return self.op(eng, lambda e: e.tensor_tensor(out=out.ap, in0=in0.ap, in1=in1.ap, op=op),
                       R=[in0.key, in1.key], W=[out.key])

    def ts(self, eng, out, in0, s1, op0, s2=None, op1=None, accum=None):
        R = [in0.key]
        W = [out.key]
        a1 = s1
        if isinstance(s1, V):
            a1 = s1.ap
            R.append(s1.key)
        a2 = s2
        if isinstance(s2, V):
            a2 = s2.ap
            R.append(s2.key)
        kw = {}
        if op1 is not None:
            kw['op1'] = op1
        if accum is not None:
            kw['accum_out'] = accum.ap
            W.append(accum.key)
        return self.op(eng, lambda e: e.tensor_scalar(out=out.ap, in0=in0.ap, scalar1=a1, scalar2=a2, op0=op0, **kw),
                       R=R, W=W)

    def stt(self, out, in0, scalar, in1, op0, op1):
        R = [in0.key, in1.key]
        a = scalar
        if isinstance(scalar, V):
            a = scalar.ap
            R.append(scalar.key)
        return self.op('dve', lambda e: e.scalar_tensor_tensor(out=out.ap, in0=in0.ap, scalar=a, in1=in1.ap,
                                                               op0=op0, op1=op1), R=R, W=[out.key])

    def cp(self, eng, out, in_):
        if eng == 'act':
            return self.op('act', lambda e: e.copy(out=out.ap, in_=in_.ap), R=[in_.key], W=[out.key])
        return self.op(eng, lambda e: e.tensor_copy(out=out.ap, in_=in_.ap), R=[in_.key], W=[out.key])

    def red(self, out, in_, op, axis=None):
        ax = axis if axis is not None else AX.X
        return self.op('dve', lambda e: e.tensor_reduce(out=out.ap, in_=in_.ap, axis=ax, op=op),
                       R=[in_.key], W=[out.key])

    def recip(self, out, in_):
        return self.op('dve', lambda e: e.reciprocal(out=out.ap, in_=in_.ap), R=[in_.key], W=[out.key])

    def memset(self, eng, out, val):
        return self.op(eng, lambda e: e.memset(out.ap, val), W=[out.key])

    def ld(self, out, src_ap, src_key=None, q='sp', **kw):
        return self.dma(q, lambda e: e.dma_start(out=out.ap, in_=src_ap, **kw), R=[src_key], W=[out.key])

    def st(self, dst_ap, dst_key, in_, q='sp', **kw):
        return self.dma(q, lambda e: e.dma_start(out=dst_ap, in_=in_.ap, **kw), R=[in_.key], W=[dst_key])


def pipeline(n_items, stages):
    leads = [ld for (_, ld) in stages]
    for t in range(-max(leads), n_items - min(leads)):
        for f, ld in stages:
            i = t + ld
            if 0 <= i < n_items:
                f(i)


class Ring:
    def __init__(self, views):
        self.views = views
        self.i = 0

    def next(self):
        v = self.views[self.i % len(self.views)]
        self.i += 1
        return v


def build(stop=None, dbg=False):
    nc = bass.Bass("TRN2", target_bir_lowering=False)

    def din(name, shape, dt=F32):
        return nc.dram_tensor(name, list(shape), dt, kind="ExternalInput").ap()

    def dscr(name, shape, dt, out=False):
        return nc.dram_tensor(name, list(shape), dt, kind=("ExternalOutput" if out else "Internal")).ap()

    x_in = din("x", [NB, T, D])
    ctx_in = din("ctx", [NB, NCX, D])
    cvec = din("cvec", [3, D])
    ada_w = din("ada_w", [DEPTH, D, 6 * D])
    ada_b = din("ada_b", [DEPTH, 6 * D])
    norm1_g = din("norm1_g", [DEPTH, D])
    w_in = din("w_in", [DEPTH, D, 2560])
    decay_f = din("decay_fwd", [DEPTH, 4])
    decay_b = din("decay_bwd", [DEPTH, 4])
    pool_w = din("pool_w", [DEPTH, 4, 128, 128])
    pool_scale = din("pool_scale", [DEPTH, 512])
    w_out = din("w_out", [DEPTH, D, D])
    norm2_g = din("norm2_g", [DEPTH, D])
    router_w = din("router_w", [DEPTH, D, 36])
    router_b = din("router_b", [DEPTH, 36])
    w_gate = din("exp_w_gate", [DEPTH, NE, D, 512])
    w_up = din("exp_w_up", [DEPTH, NE, D, 512])
    w_down = din("exp_w_down", [DEPTH, NE, 512, D])
    fin_g = din("final_norm_g", [D])
    c_rope_c = din("c_rope_c", [128, 16, 128])
    c_rope_s = din("c_rope_s", [128, 16, 128])
    c_band = din("c_band", [128, 20, 128])
    c_ident = din("c_ident", [128, 128])
    c_utri = din("c_utri", [128, 128])
    c_mtab = din("c_mtab", [128, 4, 128])
    c_pcols = din("c_pcols", [128, 8])
    c_blk = din("c_blk", [128, NBLK])

    out = nc.dram_tensor("out", [NB, T, D], F32, kind="ExternalOutput").ap()
    XS = dscr("XS", [NB, TT, D], F32, out=dbg)
    MOD = dscr("MOD", [DEPTH, 3, 6 * D], F32)
    GXKV = dscr("GXKV", [NB, TT, 2048], BF16)
    SBX = dscr("SBX", [NB, NT, 128, 512], BF16)
    HT = dscr("HT", [NB * TT, D], BF16)
    HB = dscr("HB", [NROWS, D], BF16)
    YB = dscr("YB", [NROWS, D], F32)
    WB2 = dscr("WB", [DEPTH, NE * 128, 12288], BF16)

    with ExitStack() as top:
        S = Sched(nc, top)
        bc_reg = nc.gpsimd.to_reg(DEPTH * NE * 128 - 1)

        uid = [0]

        def sb(st, name, shape, dt):
            uid[0] += 1
            nm = f"{name}_{uid[0]}"
            return V(st.enter_context(nc.sbuf_tensor(nm, list(shape), dt))[:], nm)

        def ps(st, name, shape, dt=F32):
            uid[0] += 1
            nm = f"{name}_{uid[0]}"
            S.excl.add(nm)
            return V(st.enter_context(nc.psum_tensor(nm, list(shape), dt))[:], nm)

        def ring(st, name, n, shape, dt, psum=False):
            f = ps if psum else sb
            return Ring([f(st, f"{name}{i}", shape, dt) for i in range(n)])

        ident_b = sb(top, "ident_b", [128, 128], BF16)
        ident_f = sb(top, "ident_f", [128, 128], F32)
        ones_f = sb(top, "ones_f", [128, 128], F32)
        pcols = sb(top, "pcols", [128, 8], F32)
        epsc = sb(top, "epsc", [128, 1], F32)
        S.ld(ident_f, c_ident[:, :])
        S.ld(ident_b, c_ident[:, :], q='pool')
        S.ld(pcols, c_pcols[:, :])
        S.memset('dve', ones_f, 1.0)
        S.memset('dve', epsc, EPS)

        def rstd_from_ssq(rstd, ssq, tmp, inv_n):
            S.act(tmp, ssq, AF.Sqrt, bias=epsc[0:ssq.ap.shape[0], :], scale=inv_n)
            S.recip(rstd, tmp)

        S.barrier()
        if stop == "C0":
            return nc
        conv_qs = []
        for l_ in range(DEPTH):
            q_ = []
            for e_ in range(NE):
                for part, (wsrc, cc) in enumerate(((w_gate, 8), (w_up, 8), (w_down, 4))):
                    q_.append((wsrc[l_, e_].rearrange("(c p) f -> p c f", p=128),
                               WB2[l_, e_ * 128:(e_ + 1) * 128, part * 4096:(part + 1) * 4096].rearrange("p (c f) -> p c f", c=cc),
                               ('WB', l_, e_, part)))
            conv_qs.append(q_)
        with ExitStack() as st:
            scraw = sb(st, "scraw", [128, 3, 8], F32)
            scT = sb(st, "scT", [128, 8, 32], F32)
            adab = sb(st, "adab", [32, 6 * D], F32)
            wsl = ring(st, "wsl", 4, [128, 8, 512], F32)
            mrow = ring(st, "mrow", 4, [32, 512], F32)
            pm = ring(st, "pm", 4, [128, 512], F32, psum=True)
            for r in range(3):
                S.ld(scraw[:, r, :], cvec[r].rearrange("(p c) -> p c", c=8))
            S.memset('dve', scT, 0.0)
            for r in range(3):
                S.act(scT[:, :, r], scraw[:, r, :], AF.Silu)
            for l in range(DEPTH):
                S.ld(adab, ada_b[l].partition_broadcast(32))
                awv = ada_w[l].rearrange("(p c) n -> p c n", c=8)
                for s in range(12):
                    w = wsl.next()
                    S.ld(w, awv[:, :, s * 512:(s + 1) * 512])
                    if conv_qs[0]:
                        src_, dst_, key_ = conv_qs[0].pop(0)
                        S.dma('pool', lambda en, src_=src_, dst_=dst_: en.dma_start(out=dst_, in_=src_), R=[], W=[key_])
                    p = pm.next()
                    for c in range(8):
                        S.mm(p[0:32, :], scT[:, c, :], w[:, c, :], start=(c == 0), stop=(c == 7))
                    m = mrow.next()
                    S.tt('dve', m, p[0:32, :], adab[:, s * 512:(s + 1) * 512], ALU.add)
                    S.st(MOD[l, :, s * 512:(s + 1) * 512], 'MOD', m[0:3, :])
        S.barrier()

        if stop == "L0":
            return nc

        def x_src(l, b, n):
            if l == 0:
                if n < 2:
                    return ctx_in[b, n * 128:(n + 1) * 128, :], None
                return x_in[b, (n - 2) * 128:(n - 1) * 128, :], None
            return XS[b, n * 128:(n + 1) * 128, :], ('XS', b, n)

        def load_mod_tiles(st, l, rows_segs, gvec, pfx):
            return None

        for l in range(DEPTH):
            last = (l == DEPTH - 1)
            WB = WB2[l]
            conv_q = conv_qs[l]
            conv_next = conv_qs[l + 1] if l + 1 < DEPTH else []

            def conv_issue(k, q=None):
                q = conv_q if q is None else q
                for _ in range(k):
                    if not q:
                        return
                    src_, dst_, key_ = q.pop(0)
                    S.dma('pool', lambda en, src_=src_, dst_=dst_: en.dma_start(out=dst_, in_=src_), R=[], W=[key_])

            with ExitStack() as lst:
                w_in_sb = sb(lst, "w_in_sb", [128, 8, 2560], BF16)
                w_out_sb = sb(lst, "w_out_sb", [128, 8, 1024], BF16)
                rope_c = sb(lst, "rope_c", [128, 16, 128], F32)
                rope_s = sb(lst, "rope_s", [128, 16, 128], F32)
                band = sb(lst, "band", [128, 20, 128], BF16)
                poolw = sb(lst, "poolw", [128, 4, 128], BF16)
                psc = sb(lst, "psc", [128, 4], F32)
                mtab = sb(lst, "mtab", [128, 4, 128], F32)
                maskT = sb(lst, "maskT", [128, 4, 128], F32)
                dtab = sb(lst, "dtab", [128, 6, 4], F32)
                lg = sb(lst, "lg", [128, 2, 4], F32)
                g1t = sb(lst, "g1t", [128, D], F32)
                qT = sb(lst, "qT", [128, 4, TT], BF16)
                kT = sb(lst, "kT", [128, 4, TT], BF16)

                winv = w_in[l].rearrange("(c p) n -> p c n", p=128)
                for c in range(8):
                    for j in range(5):
                        S.ld(w_in_sb[:, c, j * 512:(j + 1) * 512], winv[:, c, j * 512:(j + 1) * 512], q='pool')
                woutv = w_out[l].rearrange("(c p) n -> p c n", p=128)
                for c in range(8):
                    for j in range(2):
                        S.ld(w_out_sb[:, c, j * 512:(j + 1) * 512], woutv[:, c, j * 512:(j + 1) * 512], q='pool')
                S.ld(rope_c, c_rope_c[:, :, :])
                S.ld(rope_s, c_rope_s[:, :, :])
                S.ld(band, c_band[:, :, :], q='pool')
                S.ld(poolw, pool_w[l].rearrange("g c d -> c g d"), q='pool')
                for g in range(4):
                    S.ld(psc[:, g:g + 1], pool_scale[l, g * 128:(g + 1) * 128].rearrange("(p o) -> p o", o=1))
                S.ld(mtab, c_mtab[:, :, :])
                S.ld(g1t, norm1_g[l].partition_broadcast(128))
                S.ld(lg[:, 0, :], decay_f[l].partition_broadcast(128))
                S.ld(lg[:, 1, :], decay_b[l].partition_broadcast(128))
                S.act(lg, lg, AF.Exp, scale=-1.0)
                S.act(lg, lg, AF.Ln, bias=1.0)
                S.ts('dve', lg, lg, -1.0, ALU.mult)
                for ti, (dr, pc) in enumerate([(0, 0), (1, 1), (0, 2), (1, 3), (0, 4), (1, 4)]):
                    S.ts('dve', dtab[:, ti, :], lg[:, dr, :], pcols[:, pc:pc + 1], ALU.mult)
                S.act(dtab, dtab, AF.Exp)
                S.ts('dve', dtab[:, 0:2, :], dtab[:, 0:2, :], KS, ALU.mult)
                with ExitStack() as st:
                    e1 = sb(st, "e1", [128, 128], F32)
                    e2 = sb(st, "e2", [128, 128], F32)
                    for h in range(4):
                        S.act(e1, mtab[:, 0, :], AF.Exp, scale=lg[:, 0, h:h + 1])
                        S.tt('dve', e1, e1, mtab[:, 1, :], ALU.mult)
                        S.act(e2, mtab[:, 2, :], AF.Exp, scale=lg[:, 1, h:h + 1])
                        S.tt('dve', e2, e2, mtab[:, 3, :], ALU.mult)
                        S.tt('dve', e1, e1, e2, ALU.add)
                        S.ts('dve', maskT[:, h, :], e1, KS, ALU.mult)
                S.barrier()
                if stop == f"LS_{l}":
                    return nc

                for b in range(NB):
                    with ExitStack() as st:
                        Am = {}
                        Bm = {}
                        for r in (b, 2):
                            sc = sb(st, f"sc{r}", [128, D], F32)
                            Am[r] = sb(st, f"Am{r}", [128, D], F32)
                            Bm[r] = sb(st, f"Bm{r}", [128, D], F32)
                            S.ld(sc, MOD[l, r, 1 * D:2 * D].partition_broadcast(128), 'MOD')
                            S.ld(Bm[r], MOD[l, r, 0:D].partition_broadcast(128), 'MOD')
                            S.stt(Am[r], sc, 1.0, g1t, ALU.add, ALU.mult)
                        xt_r = ring(st, "xt", 2, [128, D], F32)
                        junk = sb(st, "junk", [128, D], BF16)
                        ssq_r = ring(st, "ssq", 2, [128, 1], F32)
                        tmp1_r = ring(st, "tmp1", 2, [128, 1], F32)
                        rstd_r = ring(st, "rstd", 2, [128, 1], F32)
                        t32_r = ring(st, "t32", 2, [128, D], F32)
                        hb_r = ring(st, "hb", 2, [128, D], BF16)
                        hT_r = ring(st, "hT", 2, [128, 8, 128], BF16)
                        r1 = sb(st, "r1", [128, 512], F32)
                        r2 = sb(st, "r2", [128, 512], F32)
                        qtm_r = ring(st, "qtm", 2, [128, 512], BF16)
                        stage_r = ring(st, "stage", 2, [128, 2048], BF16)
                        pT = ps(st, "pT", [128, 1024], BF16)
                        pP = [ps(st, f"pP{j}", [128, 512], F32) for j in range(5)]
                        pQK = ps(st, "pQK", [128, 1024], BF16)
                        cx = {}

                        def a0(n):
                            c_ = cx[n] = {}
                            conv_issue(2)
                            src, skey = x_src(l, b, n)
                            c_['xt'] = xt_r.next()
                            S.ld(c_['xt'], src, skey)

                        def a1(n):
                            c_ = cx[n]
                            r = 2 if n < 2 else b
                            xt = c_['xt']
                            ssq = ssq_r.next(); tmp1 = tmp1_r.next(); rstd = rstd_r.next()
                            t32 = t32_r.next()
                            c_['hb'] = hb_r.next()
                            S.act(junk, xt, AF.Square, accum=ssq)
                            rstd_from_ssq(rstd, ssq, tmp1, 1.0 / D)
                            S.stt(t32, xt, rstd, Am[r], ALU.mult, ALU.mult)
                            S.tt('pool', c_['hb'], t32, Bm[r], ALU.add)

                        def a2(n):
                            c_ = cx[n]
                            hb = c_['hb']
                            for c in range(8):
                                S.tr(pT[:, c * 128:(c + 1) * 128], hb[:, c * 128:(c + 1) * 128], ident_b)
                            c_['hT'] = hT_r.next()
                            S.cp('act', c_['hT'], pT.re("p (c t) -> p c t", c=8))

                        def a3(n):
                            c_ = cx[n]
                            hT = c_['hT']
                            for j in range(5):
                                for c in range(8):
                                    S.mm(pP[j], hT[:, c, :], w_in_sb[:, c, j * 512:(j + 1) * 512],
                                         start=(c == 0), stop=(c == 7))

                        def a4(n):
                            c_ = cx[n]
                            isctx = n < 2
                            stage = c_['stage'] = stage_r.next()
                            qtm = c_['qtm'] = qtm_r.next()
                            if isctx:
                                S.cp('act', qtm, pP[0])
                                S.cp('dve', stage[:, 0:512], pP[1])
                            else:
                                tn = n - 2
                                cb = rope_c[:, tn, :].bc(1, [128, 4, 128])
                                for (src_p, dst) in ((pP[0], qtm), (pP[1], stage[:, 0:512])):
                                    p3 = src_p.re("p (h d) -> p h d", h=4)
                                    p5 = src_p.re("p (h f a d) -> p h f a d", h=4, f=2, a=2)
                                    r25 = r2.re("p (h f a d) -> p h f a d", h=4, f=2, a=2)
                                    s5 = rope_s[:, tn, :].re("p (f a d) -> p f a d", f=2, a=2)
                                    S.tt('dve', r1.re("p (h d) -> p h d", h=4), p3, cb, ALU.mult)
                                    for a in range(2):
                                        S.tt('dve', r25[:, :, :, a, :], p5[:, :, :, 1 - a, :],
                                             s5[:, :, a, :].bc(1, [128, 4, 2, 32]), ALU.mult)
                                    S.tt('pool', dst, r1, r2, ALU.add)
                            S.cp('act', stage[:, 512:1024], pP[2])
                            S.act(stage[:, 1024:1536], pP[3], AF.Silu)
                            S.cp('act', stage[:, 1536:2048], pP[4])
                            S.st(GXKV[b, n * 128:(n + 1) * 128, :], ('GXKV', b, n), stage)

                        def a5(n):
                            c_ = cx[n]
                            qtm = c_['qtm']; stage = c_['stage']
                            for h in range(4):
                                S.tr(pQK[:, h * 128:(h + 1) * 128], qtm[:, h * 128:(h + 1) * 128], ident_b)
                            for h in range(4):
                                S.tr(pQK[:, 512 + h * 128:512 + (h + 1) * 128], stage[:, h * 128:(h + 1) * 128], ident_b)
                            S.cp('act', qT.sub((slice(None), slice(None), slice(n * 128, (n + 1) * 128)), ('qT', n)),
                                 pQK[:, 0:512].re("p (h t) -> p h t", h=4))
                            S.cp('dve', kT.sub((slice(None), slice(None), slice(n * 128, (n + 1) * 128)), ('kT', n)),
                                 pQK[:, 512:1024].re("p (h t) -> p h t", h=4))
                            del cx[n]

                        pipeline(NT, [(a0, 3), (a1, 2), (a2, 1), (a3, 0), (a4, 0), (a5, -1)])
                    S.barrier()
                    if stop == f"M1_{l}_{b}":
                        return nc

                    with ExitStack() as st:
                        S32 = sb(st, "S32", [128, 512], F32)
                        sbf_r = ring(st, "sbf", 2, [128, 512], BF16)
                        kv_r = ring(st, "kv", 3, [128, 1024], BF16)
                        vb_r = ring(st, "vb", 2, [128, 512], BF16)
                        pU_r = ring(st, "pU", 2, [128, 512], F32, psum=True)
                        S.memset('dve', S32, 0.0)
                        order = [1, 0] + list(range(NT - 1, 1, -1))
                        cx = {}

                        def b0(i):
                            n = order[i]
                            c_ = cx[i] = {}
                            c_['kv'] = kv_r.next()
                            S.ld(c_['kv'], GXKV[b, n * 128:(n + 1) * 128, 0:1024], ('GXKV', b, n))

                        def b1(i):
                            c_ = cx[i]
                            kv = c_['kv']
                            vb = vb_r.next()
                            S.tt('pool', vb.re("p (h e) -> p h e", h=4), kv[:, 512:1024].re("p (h e) -> p h e", h=4),
                                 dtab[:, 3, :].bc(2, [128, 4, 128]), ALU.mult)
                            pU = c_['pU'] = pU_r.next()
                            for h in range(4):
                                S.mm(pU[:, h * 128:(h + 1) * 128], kv[:, h * 128:(h + 1) * 128],
                                     vb[:, h * 128:(h + 1) * 128])

                        def b2(i):
                            n = order[i]
                            c_ = cx[i]
                            sbf = sbf_r.next()
                            S.cp('act', sbf, S32)
                            S.st(SBX[b, n, :, :], ('SBX', b, n), sbf)
                            S.tt('dve', S32.re("p (h e) -> p h e", h=4), S32.re("p (h e) -> p h e", h=4),
                                 dtab[:, 5, :].bc(2, [128, 4, 128]), ALU.mult)
                            S.tt('dve', S32, S32, c_['pU'], ALU.add)
                            del cx[i]

                        pipeline(NT, [(b0, 2), (b1, 1), (b2, 0)])
                    S.barrier()

                    with ExitStack() as st:
                        G1 = {}
                        for r in (b, 2):
                            G1[r] = sb(st, f"G1{r}", [128, D], F32)
                            S.ld(G1[r], MOD[l, r, 2 * D:3 * D].partition_broadcast(128), 'MOD')
                        S32 = sb(st, "S32f", [128, 512], F32)
                        sfb_r = ring(st, "sfb", 2, [128, 512], BF16)
                        gx_r = ring(st, "gx", 3, [128, 2048], BF16)
                        sbx_r = ring(st, "sbx", 3, [128, 512], BF16)
                        xpp_r = ring(st, "xpp", 2, [128, 512], BF16)
                        xpn_r = ring(st, "xpn", 2, [128, 512], BF16)
                        xt_r = ring(st, "xt3", 3, [128, D], F32)
                        pm_r = ring(st, "pmk", 2, [128, 512], BF16)
                        o32 = sb(st, "o32", [128, 512], F32)
                        u32 = sb(st, "u32", [128, 512], F32)
                        sq32 = sb(st, "sq32", [128, 512], F32)
                        ssqh = sb(st, "ssqh", [128, 4], F32)
                        tmph = sb(st, "tmph", [128, 4], F32)
                        rsh = sb(st, "rsh", [128, 4], F32)
                        ret_r = ring(st, "ret", 3, [128, 512], BF16)
                        mT_r = ring(st, "mT", 4, [128, 8, 128], BF16)
                        dT = sb(st, "dT", [128, 4, 128], BF16)
                        yg = sb(st, "yg", [128, D], F32)
                        xn_r = ring(st, "xn", 2, [128, D], F32)
                        vf_r = ring(st, "vf", 2, [128, 512], BF16)
                        pS = ps(st, "pS", [128, 512], F32)
                        pA = ps(st, "pA", [128, 512], F32)
                        pB = ps(st, "pB", [128, 512], F32)
                        pC = ps(st, "pC", [128, 512], F32)
                        pR = ps(st, "pR", [128, 1024], BF16)
                        pD = ps(st, "pD", [128, 512], F32)
                        pY = [ps(st, f"pY{j}", [128, 512], F32) for j in range(2)]
                        S.memset('dve', S32, 0.0)
                        sfb0 = sfb_r.next()
                        S.cp('act', sfb0, S32)
                        cx = {}
                        sfb_cur = {0: sfb0}

                        def c0(n):
                            c_ = cx[n] = {}
                            conv_issue(2)
                            isctx = n < 2
                            c_['do_out'] = not (last and isctx)
                            c_['first'] = n in (0, 2)
                            c_['lastt'] = n in (1, NT - 1)
                            gx = c_['gx'] = gx_r.next()
                            S.ld(gx, GXKV[b, n * 128:(n + 1) * 128, :], ('GXKV', b, n))
                            if c_['do_out']:
                                c_['sbx'] = sbx_r.next()
                                S.ld(c_['sbx'], SBX[b, n, :, :], ('SBX', b, n))
                                c_['xpp'] = c_['xpn'] = None
                                if not c_['first']:
                                    c_['xpp'] = xpp_r.next()
                                    S.ld(c_['xpp'], GXKV[b, (n - 1) * 128:n * 128, 1536:2048], ('GXKV', b, n - 1))
                                if not c_['lastt']:
                                    c_['xpn'] = xpn_r.next()
                                    S.ld(c_['xpn'], GXKV[b, (n + 1) * 128:(n + 2) * 128, 1536:2048], ('GXKV', b, n + 1))

                        def c0b(n):
                            c_ = cx[n]
                            if c_['do_out']:
                                c_['xt'] = xt_r.next()
                                src, skey = x_src(l, b, n)
                                S.ld(c_['xt'], src, skey)

                        def c1(n):
                            c_ = cx[n]
                            gx = c_['gx']
                            vf = c_['vf'] = vf_r.next()
                            S.tt('pool', vf.re("p (h e) -> p h e", h=4), gx[:, 512:1024].re("p (h e) -> p h e", h=4),
                                 dtab[:, 2, :].bc(2, [128, 4, 128]), ALU.mult)
                            if not c_['do_out']:
                                return
                            qTn = qT.sub((slice(None), slice(None), slice(n * 128, (n + 1) * 128)), ('qT', n))
                            kTn = kT.sub((slice(None), slice(None), slice(n * 128, (n + 1) * 128)), ('kT', n))
                            for h in range(4):
                                S.mm(pS[:, h * 128:(h + 1) * 128], kTn[:, h, :], qTn[:, h, :])
                            pmk = c_['pmk'] = pm_r.next()
                            S.tt('dve', pmk, pS, maskT.re("p h i -> p (h i)"), ALU.mult)
                            mT = c_['mT'] = mT_r.next()
                            xpp = c_['xpp']; xpn = c_['xpn']
                            for g in range(4):
                                terms = []
                                if xpp is not None:
                                    terms.append((xpp[:, g * 128:(g + 1) * 128], band[:, g * 5 + 0, :]))
                                kind = 3 if c_['first'] else (4 if c_['lastt'] else 1)
                                terms.append((gx[:, 1536 + g * 128:1536 + (g + 1) * 128], band[:, g * 5 + kind, :]))
                                if xpn is not None:
                                    terms.append((xpn[:, g * 128:(g + 1) * 128], band[:, g * 5 + 2, :]))
                                for ti, (a_, b_) in enumerate(terms):
                                    S.mm(pD[:, g * 128:(g + 1) * 128], a_, b_, start=(ti == 0),
                                         stop=(ti == len(terms) - 1))
                            S.cp('act', dT, pD.re("p (g t) -> p g t", g=4))
                            for g in range(4):
                                S.mm(pD[:, g * 128:(g + 1) * 128], poolw[:, g, :], dT[:, g, :])
                            S.tt('dve', mT[:, 4:8, :], pD.re("p (g t) -> p g t", g=4),
                                 psc.bc(2, [128, 4, 128]), ALU.mult)

                        def c2(n):
                            c_ = cx[n]
                            gx = c_['gx']
                            ksl = gx[:, 0:512]
                            vsl = gx[:, 512:1024]
                            sfb = sfb_cur[n]
                            if c_['do_out']:
                                qTn = qT.sub((slice(None), slice(None), slice(n * 128, (n + 1) * 128)), ('qT', n))
                                pmk = c_['pmk']; sbx = c_['sbx']
                                for h in range(4):
                                    S.mm(pA[:, h * 128:(h + 1) * 128], pmk[:, h * 128:(h + 1) * 128],
                                         vsl[:, h * 128:(h + 1) * 128])
                                for h in range(4):
                                    S.mm(pB[:, h * 128:(h + 1) * 128], qTn[:, h, :], sfb[:, h * 128:(h + 1) * 128])
                                for h in range(4):
                                    S.mm(pC[:, h * 128:(h + 1) * 128], qTn[:, h, :], sbx[:, h * 128:(h + 1) * 128])
                            vf = c_['vf']
                            for h in range(4):
                                S.mm(pS[:, h * 128:(h + 1) * 128], ksl[:, h * 128:(h + 1) * 128],
                                     vf[:, h * 128:(h + 1) * 128])
                            S.tt('dve', S32.re("p (h e) -> p h e", h=4), S32.re("p (h e) -> p h e", h=4),
                                 dtab[:, 4, :].bc(2, [128, 4, 128]), ALU.mult)
                            S.tt('dve', S32, S32, pS, ALU.add)
                            sfbn = sfb_r.next()
                            S.cp('act', sfbn, S32)
                            sfb_cur[n + 1] = sfbn
                            if c_['do_out']:
                                S.tt('dve', o32.re("p (h e) -> p h e", h=4), pB.re("p (h e) -> p h e", h=4),
                                     dtab[:, 0, :].bc(2, [128, 4, 128]), ALU.mult)
                                S.tt('dve', u32.re("p (h e) -> p h e", h=4), pC.re("p (h e) -> p h e", h=4),
                                     dtab[:, 1, :].bc(2, [128, 4, 128]), ALU.mult)
                                S.tt('pool', o32, o32, u32, ALU.add)
                                S.tt('dve', o32, o32, pA, ALU.add)
                                S.act(sq32, o32, AF.Square)
                                S.red(ssqh, sq32.re("p (h e) -> p h e", h=4), ALU.add)
                                rstd_from_ssq(rsh, ssqh, tmph, 1.0 / 128)
                                S.tt('pool', u32.re("p (h e) -> p h e", h=4), o32.re("p (h e) -> p h e", h=4),
                                     rsh.bc(2, [128, 4, 128]), ALU.mult)
                                ret = c_['ret'] = ret_r.next()
                                S.tt('pool', ret, u32, gx[:, 1024:1536], ALU.mult)

                        def c3(n):
                            c_ = cx[n]
                            if c_['do_out']:
                                r = 2 if n < 2 else b
                                ret = c_['ret']; mT = c_['mT']; xt = c_['xt']
                                for h in range(4):
                                    S.tr(pR[:, h * 128:(h + 1) * 128], ret[:, h * 128:(h + 1) * 128], ident_b)
                                S.cp('act', mT[:, 0:4, :], pR[:, 0:512].re("p (h t) -> p h t", h=4))
                                for j in range(2):
                                    for c in range(8):
                                        S.mm(pY[j], mT[:, c, :], w_out_sb[:, c, j * 512:(j + 1) * 512],
                                             start=(c == 0), stop=(c == 7))
                                for j in range(2):
                                    S.tt('dve', yg[:, j * 512:(j + 1) * 512], pY[j], G1[r][:, j * 512:(j + 1) * 512],
                                         ALU.mult)
                                xn = xn_r.next()
                                S.tt('pool', xn, xt, yg, ALU.add)
                                S.st(XS[b, n * 128:(n + 1) * 128, :], ('XS', b, n), xn)
                            del cx[n]

                        pipeline(NT, [(c0, 2), (c1, 1), (c0b, 0), (c2, 0), (c3, -2)])
                    S.barrier()
                    if stop == f"M3_{l}_{b}":
                        return nc
            S.barrier()
            if stop == f"MIX_{l}":
                return nc

            conv_issue(len(conv_q))
            S.barrier()
            tiles = [(b, n) for b in range(NB) for n in range(NT) if not (last and n < 2)]
            NG = len(tiles)
            with ExitStack() as lst:
                OH1 = sb(lst, "OH1", [128, NG, 32], F32)
                OH2 = sb(lst, "OH2", [128, NG, 32], F32)
                W12 = sb(lst, "W12", [128, NG, 2], F32)
                RK = sb(lst, "RK", [128, NG, 2], F32)
                DEST = sb(lst, "DEST", [128, NG, 2], I32)
                widx = sb(lst, "widx", [128, NBLK], I32)
                g2t = sb(lst, "g2t", [128, D], F32)
                pstart = sb(lst, "pstart", [128, 32], F32)
                S.ld(g2t, norm2_g[l].partition_broadcast(128))
                rows = sorted(set(2 if n < 2 else b for (b, n) in tiles))
                with ExitStack() as st:
                    Am = {}
                    Bm = {}
                    for r in rows:
                        sc = sb(st, f"sc2{r}", [128, D], F32)
                        Am[r] = sb(st, f"A2{r}", [128, D], F32)
                        Bm[r] = sb(st, f"B2{r}", [128, D], F32)
                        S.ld(sc, MOD[l, r, 4 * D:5 * D].partition_broadcast(128), 'MOD')
                        S.ld(Bm[r], MOD[l, r, 3 * D:4 * D].partition_broadcast(128), 'MOD')
                        S.stt(Am[r], sc, 1.0, g2t, ALU.add, ALU.mult)
                    wr = sb(st, "wr", [128, 8, 36], F32)
                    rbias = sb(st, "rbias", [128, 36], F32)
                    utri = sb(st, "utri", [128, 128], F32)
                    Racc = sb(st, "Racc", [128, 32], F32)
                    zt = sb(st, "zt", [128, 4096], BF16)
                    S.memset('pool', zt, 0.0)
                    HBv = HB.rearrange("(p a) d -> p (a d)", p=128)
                    for kz in range(NBLK * D // 4096):
                        S.st(HBv[:, kz * 4096:(kz + 1) * 4096], 'HB', zt)
                    S.ld(wr, router_w[l].rearrange("(c p) n -> p c n", p=128))
                    S.ld(rbias, router_b[l].partition_broadcast(128))
                    S.ld(utri, c_utri[:, :])
                    S.memset('dve', Racc, 0.0)
                    xt_r = ring(st, "xtr", 2, [128, D], F32)
                    junk = sb(st, "junkr", [128, D], BF16)
                    ssq = sb(st, "ssqr", [128, 1], F32)
                    tmp1 = sb(st, "tmp1r", [128, 1], F32)
                    rstd = sb(st, "rstdr", [128, 1], F32)
                    t32 = sb(st, "t32r", [128, D], F32)
                    h32_r = ring(st, "h32", 2, [128, D], F32)
                    hbp_r = ring(st, "hbp", 2, [128, D], BF16)
                    h32T = sb(st, "h32T", [128, 8, 128], F32)
                    lgt = sb(st, "lgt", [128, 36], F32)
                    sm = sb(st, "sm", [128, 16], F32)
                    ohg = sb(st, "ohg", [128, 4], F32)
                    j4 = sb(st, "j4", [128, 4], F32)
                    sel = sb(st, "sel", [128, 4, 8], F32)
                    ein = sb(st, "ein", [128, 8], F32)
                    m8 = sb(st, "m8", [128, 8], F32)
                    eq1 = sb(st, "eq1", [128, 8], F32)
                    eq2 = sb(st, "eq2", [128, 8], F32)
                    OHs = sb(st, "OHs", [128, 32], F32)
                    t32b = sb(st, "t32b", [128, 32], F32)
                    pTr = [ps(st, f"pTr{j}", [128, 512], F32) for j in range(2)]
                    pL = ps(st, "pL", [128, 512], F32)
                    pCn = ps(st, "pCn", [128, 512], F32)
                    cx = {}

                    def r0(gt):
                        b, n = tiles[gt]
                        c_ = cx[gt] = {}
                        conv_issue(2, conv_next)
                        c_['xt'] = xt_r.next()
                        S.ld(c_['xt'], XS[b, n * 128:(n + 1) * 128, :], ('XS', b, n))

                    def r1(gt):
                        b, n = tiles[gt]
                        c_ = cx[gt]
                        r = 2 if n < 2 else b
                        xt = c_['xt']
                        S.act(junk, xt, AF.Square, accum=ssq)
                        S.act(tmp1, ssq, AF.Ln, bias=epsc, scale=1.0 / D)
                        S.act(rstd, tmp1, AF.Exp, scale=-0.5)
                        S.stt(t32, xt, rstd, Am[r], ALU.mult, ALU.mult)
                        h32 = c_['h32'] = h32_r.next()
                        S.tt('pool', h32, t32, Bm[r], ALU.add)
                        hbp = hbp_r.next()
                        S.cp('act', hbp, h32)
                        S.st(HT[gt * 128:(gt + 1) * 128, :], ('HT', gt), hbp)

                    def r2(gt):
                        c_ = cx[gt]
                        h32 = c_['h32']
                        for c in range(8):
                            S.tr(pTr[c // 4][:, (c % 4) * 128:(c % 4 + 1) * 128], h32[:, c * 128:(c + 1) * 128], ident_f)
                        S.cp('dve', h32T[:, 0:4, :], pTr[0].re("p (c t) -> p c t", c=4))
                        S.cp('act', h32T[:, 4:8, :], pTr[1].re("p (c t) -> p c t", c=4))
                        for c in range(8):
                            S.mm(pL[:, 0:36], h32T[:, c, :], wr[:, c, :], start=(c == 0), stop=(c == 7))
                        S.tt('dve', lgt, pL[:, 0:36], rbias, ALU.add)
                        gl = lgt[:, 0:4]
                        S.red(sm[:, 0:1], gl, ALU.max)
                        S.ts('dve', ohg, gl, sm[:, 0:1], ALU.is_equal)
                        S.ts('dve', sm[:, 1:2], sm[:, 0:1], -1.0, ALU.mult)
                        S.act(j4, gl, AF.Exp, bias=sm[:, 1:2], accum=sm[:, 2:3])
                        S.recip(sm[:, 3:4], sm[:, 2:3])
                        S.tt('dve', sel, lgt[:, 4:36].re("p (g e) -> p g e", g=4), ohg.bc(2, [128, 4, 8]), ALU.mult)
                        S.red(ein, sel.re("p g e -> p e g"), ALU.add)
                        S.op('dve', lambda e: e.max(out=m8.ap, in_=ein.ap), R=[ein.key], W=[m8.key])
                        S.ts('dve', eq1, ein, m8[:, 0:1], ALU.is_equal)
                        S.ts('dve', eq2, ein, m8[:, 1:2], ALU.is_equal)
                        S.tt('dve', sm[:, 4:5], m8[:, 1:2], m8[:, 0:1], ALU.subtract)
                        S.act(sm[:, 5:6], sm[:, 4:5], AF.Exp)
                        S.ts('dve', sm[:, 6:7], sm[:, 5:6], 1.0, ALU.add)
                        S.recip(sm[:, 7:8], sm[:, 6:7])
                        w12 = W12.sub((slice(None), gt, slice(None)), ('W12', gt))
                        S.tt('dve', w12[:, 0:1], sm[:, 7:8], sm[:, 3:4], ALU.mult)
                        S.tt('dve', w12[:, 1:2], w12[:, 0:1], sm[:, 5:6], ALU.mult)
                        oh1 = OH1.sub((slice(None), gt, slice(None)), ('OH1', gt))
                        oh2 = OH2.sub((slice(None), gt, slice(None)), ('OH2', gt))
                        S.tt('dve', oh1.re("p (g e) -> p g e", g=4), ohg.bc(2, [128, 4, 8]), eq1.bc(1, [128, 4, 8]), ALU.mult)
                        S.tt('dve', oh2.re("p (g e) -> p g e", g=4), ohg.bc(2, [128, 4, 8]), eq2.bc(1, [128, 4, 8]), ALU.mult)
                        S.tt('dve', OHs, oh1, oh2, ALU.add)
                        S.mm(pCn[:, 0:32], utri, OHs, start=True, stop=False)
                        S.mm(pCn[:, 0:32], ones_f, Racc, start=False, stop=True)
                        rk = RK.sub((slice(None), gt, slice(None)), ('RK', gt))
                        S.tt('dve', t32b, oh1, pCn[:, 0:32], ALU.mult)
                        S.red(rk[:, 0:1], t32b, ALU.add)
                        S.tt('dve', t32b, oh2, pCn[:, 0:32], ALU.mult)
                        S.red(rk[:, 1:2], t32b, ALU.add)
                        S.tt('dve', Racc, Racc, OHs, ALU.add)
                        del cx[gt]

                    pipeline(NG, [(r0, 2), (r1, 1), (r2, 0)])
                    cnt = sb(st, "cnt", [128, 32], F32)
                    cnti = sb(st, "cnti", [128, 32], I32)
                    padded = sb(st, "padded", [128, 32], F32)
                    ca = sb(st, "ca", [128, 32], F32)
                    cb_ = sb(st, "cb_", [128, 32], F32)
                    cmp = sb(st, "cmp", [128, NBLK, 32], F32)
                    bef = sb(st, "bef", [128, NBLK], F32)
                    same = sb(st, "same", [128, NBLK], F32)
                    blk = sb(st, "blk", [128, NBLK], F32)
                    S.ld(blk, c_blk[:, :])
                    S.mm(pCn[:, 0:32], ones_f, Racc)
                    S.cp('dve', cnt, pCn[:, 0:32])
                    S.ts('dve', cnti, cnt, 127.0, ALU.add)
                    S.ts('dve', cnti, cnti, 7, ALU.arith_shift_right, s2=7, op1=ALU.logical_shift_left)
                    S.cp('dve', padded, cnti)
                    cur, oth = ca, cb_
                    S.cp('dve', cur, padded)
                    for s in (1, 2, 4, 8, 16):
                        S.cp('dve', oth, cur)
                        S.tt('dve', oth[:, s:32], cur[:, s:32], cur[:, 0:32 - s], ALU.add)
                        cur, oth = oth, cur
                    pends = cur
                    S.tt('dve', pstart, pends, padded, ALU.subtract)
                    S.tt('dve', cmp, pends.bc(1, [128, NBLK, 32]), blk.bc(2, [128, NBLK, 32]), ALU.is_le)
                    S.red(bef, cmp, ALU.add)
                    S.ts('dve', bef, bef, 31.0, ALU.min)
                    S.ts('dve', bef, bef, 128.0, ALU.mult, s2=pcols[:, 3:4], op1=ALU.add)
                    S.memset('dve', same, 0.0)
                    S.tt('dve', same[:, 1:NBLK], bef[:, 1:NBLK], bef[:, 0:NBLK - 1], ALU.is_equal)
                    for qq in range(1, NSTREAM):
                        S.memset('dve', same[:, qq * BPS:qq * BPS + 1], 0.0)
                    S.ts('dve', bef, bef, float(l * NE * 128), ALU.add)
                    S.stt(bef, same, float(BIGIDX), bef, ALU.mult, ALU.add)
                    S.cp('dve', widx, bef)
                    hb2_r = ring(st, "hb2", 2, [128, D], BF16)
                    dsf = sb(st, "dsf", [128, 2], F32)
                    for gt, (b, n) in enumerate(tiles):
                        rk = RK.sub((slice(None), gt, slice(None)), ('RK', gt))
                        dst = DEST.sub((slice(None), gt, slice(None)), ('DEST', gt))
                        for k, OHk in enumerate((OH1, OH2)):
                            ohk = OHk.sub((slice(None), gt, slice(None)), (OHk.key, gt))
                            S.tt('dve', t32b, ohk, pstart, ALU.mult)
                            S.red(dsf[:, k:k + 1], t32b, ALU.add)
                        S.tt('dve', dsf, dsf, rk, ALU.add)
                        S.cp('dve', dst, dsf)
                        hb2 = hb2_r.next()
                        S.ld(hb2, HT[gt * 128:(gt + 1) * 128, :], ('HT', gt))
                        for k in range(2):
                            S.dma('pool', lambda e, k=k, dst=dst, hb2=hb2: e.indirect_dma_start(
                                out=HB[:, :], out_offset=bass.IndirectOffsetOnAxis(ap=dst.ap[:, k:k + 1], axis=0),
                                in_=hb2.ap, in_offset=None), R=[hb2.key, dst.key], W=['HB'])
                S.barrier()
                if stop == f"R_{l}":
                    return nc

                with ExitStack() as st:
                    wb_r = ring(st, "wbuf", NSTREAM, [128, 12288], BF16)
                    hg_r = ring(st, "hg", 5, [128, D], BF16)
                    hgT_r = ring(st, "hgT", 2, [128, 8, 128], BF16)
                    sg_r = ring(st, "sg", 2, [128, 512], F32)
                    actb_r = ring(st, "actb", 2, [128, 512], BF16)
                    actT_r = ring(st, "actT", 2, [128, 4, 128], BF16)
                    yb_r = ring(st, "yb", 2, [128, D], F32)
                    pT = ps(st, "pTe", [128, 1024], BF16)
                    pG_r = ring(st, "pG", 2, [128, 512], F32, psum=True)
                    pUp_r = ring(st, "pUp", 2, [128, 512], F32, psum=True)
                    pT2 = ps(st, "pT2", [128, 1024], BF16)
                    pY = [ps(st, f"pYe{j}", [128, 512], F32) for j in range(2)]
                    cx = {}

                    def e_s0(j):
                        i = (j % NSTREAM) * BPS + j // NSTREAM
                        c_ = cx[j] = {'i': i}
                        wb_ = wb_r.next()
                        S.dma('pool', lambda e, wb_=wb_, i=i: e.indirect_dma_start(
                            out=wb_.ap, out_offset=None, in_=WB2.rearrange("l r x -> (l r) x"),
                            in_offset=bass.IndirectOffsetOnAxis(ap=widx.ap[:, i:i + 1], axis=0),
                            bounds_check=bc_reg, oob_is_err=False),
                            R=[widx.key], W=[wb_.key])
                        c_['wg3'] = wb_[:, 0:4096].re("p (c f) -> p c f", c=8)
                        c_['wu3'] = wb_[:, 4096:8192].re("p (c f) -> p c f", c=8)
                        c_['wd'] = wb_[:, 8192:12288].re("p (c f) -> p c f", c=4)
                        c_['hg'] = hg_r.next()
                        S.ld(c_['hg'], HB[i * 128:(i + 1) * 128, :], 'HB')

                    def e_s1(j):
                        c_ = cx[j]
                        hg = c_['hg']
                        for c in range(8):
                            S.tr(pT[:, c * 128:(c + 1) * 128], hg[:, c * 128:(c + 1) * 128], ident_b)
                        c_['hgT'] = hgT_r.next()
                        S.cp('act', c_['hgT'], pT.re("p (c t) -> p c t", c=8))

                    def e_s2(j):
                        c_ = cx[j]
                        c_['pG'] = pG_r.next()
                        c_['pUp'] = pUp_r.next()
                        for c in range(8):
                            S.mm(c_['pG'], c_['hgT'][:, c, :], c_['wg3'][:, c, :], start=(c == 0), stop=(c == 7))
                        for c in range(8):
                            S.mm(c_['pUp'], c_['hgT'][:, c, :], c_['wu3'][:, c, :], start=(c == 0), stop=(c == 7))

                    def e_s3(j):
                        c_ = cx[j]
                        sg = sg_r.next()
                        c_['actb'] = actb_r.next()
                        S.act(sg, c_['pG'], AF.Silu)
                        S.tt('dve', c_['actb'], sg, c_['pUp'], ALU.mult)

                    def e_s4(j):
                        c_ = cx[j]
                        for c in range(4):
                            S.tr(pT2[:, c * 128:(c + 1) * 128], c_['actb'][:, c * 128:(c + 1) * 128], ident_b)
                        c_['actT'] = actT_r.next()
                        S.cp('act', c_['actT'], pT2[:, 0:512].re("p (c t) -> p c t", c=4))

                    def e_s5(j):
                        c_ = cx[j]
                        for jj in range(2):
                            for c in range(4):
                                S.mm(pY[jj], c_['actT'][:, c, :], c_['wd'][:, c, jj * 512:(jj + 1) * 512],
                                     start=(c == 0), stop=(c == 3))
                        yb = yb_r.next()
                        S.cp('dve', yb[:, 0:512], pY[0])
                        S.cp('act', yb[:, 512:1024], pY[1])
                        i = c_['i']
                        S.st(YB[i * 128:(i + 1) * 128, :], 'YB', yb)
                        del cx[j]

                    pipeline(NBLK, [(e_s0, 4), (e_s1, 1), (e_s2, 0), (e_s3, 0), (e_s4, -1), (e_s5, -1)])
                S.barrier()
                if stop == f"E_{l}":
                    return nc

                with ExitStack() as st:
                    G2 = {}
                    for r in rows:
                        G2[r] = sb(st, f"G2{r}", [128, D], F32)
                        S.ld(G2[r], MOD[l, r, 5 * D:6 * D].partition_broadcast(128), 'MOD')
                    fg = sb(st, "fg", [128, D], F32)
                    S.ld(fg, fin_g.partition_broadcast(128))
                    y1_r = ring(st, "y1", 3, [128, D], F32)
                    y2_r = ring(st, "y2", 3, [128, D], F32)
                    xt_r = ring(st, "xtf", 3, [128, D], F32)
                    ya = sb(st, "ya", [128, D], F32)
                    xn_r = ring(st, "xnf", 2, [128, D], F32)
                    junk = sb(st, "junkf", [128, D], BF16)
                    ssq = sb(st, "ssqf", [128, 1], F32)
                    tmp1 = sb(st, "tmp1f", [128, 1], F32)
                    rstd = sb(st, "rstdf", [128, 1], F32)
                    ot_r = ring(st, "ot", 2, [128, D], F32)
                    cx = {}

                    def f0(gt):
                        b, n = tiles[gt]
                        c_ = cx[gt] = {}
                        dst = DEST.sub((slice(None), gt, slice(None)), ('DEST', gt))
                        c_['y1'] = y1_r.next()
                        c_['y2'] = y2_r.next()
                        for k, yk in enumerate((c_['y1'], c_['y2'])):
                            S.dma('pool', lambda e, k=k, yk=yk, dst=dst: e.indirect_dma_start(
                                out=yk.ap, out_offset=None, in_=YB[:, :],
                                in_offset=bass.IndirectOffsetOnAxis(ap=dst.ap[:, k:k + 1], axis=0)),
                                R=['YB', dst.key], W=[yk.key])
                        c_['xt'] = xt_r.next()
                        S.ld(c_['xt'], XS[b, n * 128:(n + 1) * 128, :], ('XS', b, n))

                    def f1(gt):
                        b, n = tiles[gt]
                        c_ = cx[gt]
                        r = 2 if n < 2 else b
                        w12 = W12.sub((slice(None), gt, slice(None)), ('W12', gt))
                        y1 = c_['y1']; y2 = c_['y2']; xt = c_['xt']
                        S.act(ya, y1, AF.Copy, scale=w12[:, 0:1])
                        S.stt(ya, y2, w12[:, 1:2], ya, ALU.mult, ALU.add)
                        S.tt('dve', ya, ya, G2[r], ALU.mult)
                        xn = xn_r.next()
                        S.tt('dve', xn, xt, ya, ALU.add)
                        if last:
                            S.act(junk, xn, AF.Square, accum=ssq)
                            rstd_from_ssq(rstd, ssq, tmp1, 1.0 / D)
                            ot = ot_r.next()
                            S.stt(ot, xn, rstd, fg, ALU.mult, ALU.mult)
                            S.st(out[b, (n - 2) * 128:(n - 1) * 128, :], ('out', b, n), ot)
                        else:
                            S.st(XS[b, n * 128:(n + 1) * 128, :], ('XS', b, n), xn)
                        del cx[gt]

                    pipeline(NG, [(f0, 2), (f1, 0)])
                S.barrier()
        S.barrier(engines=['sp'])
    return nc


def _consts():
    c = {}
    t = np.arange(T)
    rows = (t // 64).astype(np.float32)
    cols = (t % 64).astype(np.float32)
    inv = (10000.0 ** (-np.arange(32, dtype=np.float32) / 32)).astype(np.float32)
    ar = rows[:, None] * inv[None, :]
    ac = cols[:, None] * inv[None, :]
    C = np.concatenate([np.cos(ar), np.cos(ar), np.cos(ac), np.cos(ac)], axis=1)
    Sg = np.concatenate([-np.sin(ar), np.sin(ar), -np.sin(ac), np.sin(ac)], axis=1)
    c["c_rope_c"] = np.ascontiguousarray(C.reshape(16, 128, 128).transpose(1, 0, 2)).astype(np.float32)
    c["c_rope_s"] = np.ascontiguousarray(Sg.reshape(16, 128, 128).transpose(1, 0, 2)).astype(np.float32)
    band = np.zeros((128, 20, 128), np.float32)
    tp = np.arange(128)[:, None]
    tt = np.arange(128)[None, :]
    for g, w in enumerate((2, 4, 8, 16)):
        h = w // 2
        eye = (tp == tt).astype(np.float32)
        band[:, g * 5 + 0, :] = ((tp - 128) >= (tt - h)).astype(np.float32) / w
        band[:, g * 5 + 2, :] = ((tp + 128) <= (tt + h - 1)).astype(np.float32) / w
        inwin = ((tp >= tt - h) & (tp <= tt + h - 1)).astype(np.float32)
        band[:, g * 5 + 1, :] = inwin / w - eye
        cnt_first = (np.minimum(tt + h, 10 ** 9) - np.maximum(tt - h, 0)).astype(np.float32)
        band[:, g * 5 + 3, :] = inwin / cnt_first - eye
        cnt_last = (np.minimum(tt + h, 128) - (tt - h)).astype(np.float32)
        band[:, g * 5 + 4, :] = inwin / cnt_last - eye
    c["c_band"] = band
    c["c_ident"] = np.eye(128, dtype=np.float32)
    c["c_utri"] = (tp < tt).astype(np.float32)
    j = np.arange(128)[:, None].astype(np.float32)
    i = np.arange(128)[None, :].astype(np.float32)
    mt = np.zeros((128, 4, 128), np.float32)
    mt[:, 0, :] = np.maximum(i - j, 0)
    mt[:, 1, :] = (i >= j)
    mt[:, 2, :] = np.maximum(j - i, 0)
    mt[:, 3, :] = (j >= i)
    c["c_mtab"] = mt
    p = np.arange(128, dtype=np.float32)
    pc = np.zeros((128, 8), np.float32)
    pc[:, 0] = p + 1
    pc[:, 1] = 128 - p
    pc[:, 2] = 127 - p
    pc[:, 3] = p
    pc[:, 4] = 128
    c["c_pcols"] = pc
    c["c_blk"] = np.tile((np.arange(NBLK, dtype=np.float32) * 128)[None, :], (128, 1))
    return c


_NC_CACHE = {}


def _prep_inputs(inputs):
    f = lambda a: np.ascontiguousarray(np.asarray(a, dtype=np.float32))
    x = f(inputs["x"])
    ctx = f(inputs["ctx"])
    c = f(inputs["c"])
    c_ctx = f(inputs["c_ctx"])
    shared = {k: f(inputs[k]) for k in ("ada_w", "ada_b", "norm1_g", "w_in", "decay_fwd", "decay_bwd", "pool_w",
                                        "pool_scale", "w_out", "norm2_g", "exp_w_gate", "exp_w_up", "exp_w_down",
                                        "final_norm_g")}
    shared["router_w"] = np.ascontiguousarray(np.concatenate([f(inputs["router_g_w"]), f(inputs["router_e_w"])], axis=2))
    shared["router_b"] = np.ascontiguousarray(np.concatenate([f(inputs["router_g_b"]), f(inputs["router_e_b"])], axis=1))
    shared.update(_consts())
    in_maps = []
    for i in range(8):
        m = dict(shared)
        m["x"] = np.ascontiguousarray(x[2 * i:2 * i + 2])
        m["ctx"] = np.ascontiguousarray(ctx[2 * i:2 * i + 2])
        m["cvec"] = np.ascontiguousarray(np.stack([c[2 * i], c[2 * i + 1], c_ctx], axis=0))
        in_maps.append(m)
    return in_maps


def kernel(**inputs):
    in_maps = _prep_inputs(inputs)
    if "nc" not in _NC_CACHE:
        _NC_CACHE["nc"] = build()
    nc = _NC_CACHE["nc"]
    res = run_bass_kernel_spmd(nc, in_maps, core_ids=list(range(8)))
    return np.concatenate([np.asarray(r["out"], dtype=np.float32) for r in res.results], axis=0)
```

```python
import os
import numpy as np
from contextlib import ExitStack
import concourse.bass as bass
import concourse.mybir as mybir
from concourse.bass_utils import run_bass_kernel_spmd

F32 = mybir.dt.float32
BF16 = mybir.dt.bfloat16
I32 = mybir.dt.int32
AF = mybir.ActivationFunctionType
ALU = mybir.AluOpType
AX = mybir.AxisListType

D = 1024
T = 2048
NCX = 256
TT = T + NCX
NT = TT // 128
NB = 2
DEPTH = 2
NE = 32
NBLK = 108
NROWS = NBLK * 128
NSTREAM = 6
BPS = NBLK // NSTREAM
BIGIDX = 1 << 20
EPS = 1e-6
KS = 128.0 ** -0.5


class V:
    __slots__ = ("ap", "key")

    def __init__(self, ap, key):
        self.ap = ap
        self.key = key

    def __getitem__(self, idx):
        return V(self.ap[idx], self.key)

    def sub(self, idx, key):
        return V(self.ap[idx], key)

    def re(self, pat, **kw):
        return V(self.ap.rearrange(pat, **kw), self.key)

    def bc(self, axis, shape):
        return V(self.ap.unsqueeze(axis).to_broadcast(list(shape)), self.key)


class Sched:
    def __init__(self, nc, stack, n_dma_sems=8):
        self.nc = nc
        self.eng = {'pe': nc.tensor, 'dve': nc.vector, 'act': nc.scalar, 'pool': nc.gpsimd, 'sp': nc.sync}
        self.sem = {}
        self.cnt = {}
        self.waited = {e: {} for e in self.eng}
        for e in self.eng:
            self.sem[e] = stack.enter_context(nc.semaphore(f"s_{e}"))
            self.cnt[e] = 0
        self.dsem = {}
        self.dval = {}
        self.dnext = {}
        for q, nq in (('sp', 12), ('pool', 24)):
            self.dsem[q] = [stack.enter_context(nc.semaphore(f"d_{q}{i}")) for i in range(nq)]
            self.dval[q] = [0] * nq
            self.dnext[q] = 0
        self.last_w = {}
        self.readers = {}
        self.n_ins = 0
        self.excl = set()

    def _deps(self, reads, writes):
        deps = []
        for k in reads:
            if k in self.last_w:
                deps.append(self.last_w[k])
        for k in writes:
            if k in self.last_w:
                deps.append(self.last_w[k])
            deps.extend(self.readers.get(k, ()))
        return deps

    def _wait(self, e, deps):
        w = self.waited[e]
        need = {}
        for (sid, sem, val) in deps:
            if e == 'pe' and sid == 'c_pe':
                continue
            if w.get(sid, 0) >= val:
                continue
            if sid not in need or need[sid][1] < val:
                need[sid] = (sem, val)
        for sid, (sem, val) in need.items():
            self.eng[e].wait_ge(sem, val)
            w[sid] = val
            self.n_ins += 1

    def _commit(self, tok, reads, writes):
        for k in reads:
            self.readers.setdefault(k, []).append(tok)
        for k in writes:
            self.last_w[k] = tok
            self.readers[k] = []

    def op(self, e, fn, R=(), W=()):
        W = [k for k in W if k is not None] + [k for k in R if k in self.excl]
        R = [k for k in R if k is not None and k not in self.excl]
        self._wait(e, self._deps(R, W))
        ins = fn(self.eng[e])
        self.cnt[e] += 1
        ins.then_inc(self.sem[e], 1)
        tok = ('c_' + e, self.sem[e], self.cnt[e])
        self._commit(tok, R, W)
        self.n_ins += 1
        return tok

    def dma(self, q, fn, R=(), W=()):
        R = [k for k in R if k is not None]
        W = [k for k in W if k is not None]
        deps = self._deps(R, W)
        i = self.dnext[q]
        self.dnext[q] = (i + 1) % len(self.dsem[q])
        sem = self.dsem[q][i]
        sid = f'd_{q}{i}'
        if self.dval[q][i] > 0:
            deps.append((sid, sem, self.dval[q][i]))
        self._wait(q, deps)
        ins = fn(self.eng[q])
        self.dval[q][i] += 16
        ins.then_inc(sem, 16)
        tok = (sid, sem, self.dval[q][i])
        self._commit(tok, R, W)
        self.n_ins += 1
        return tok

    def all_tokens(self):
        toks = []
        for e in self.eng:
            if self.cnt[e] > 0:
                toks.append(('c_' + e, self.sem[e], self.cnt[e]))
        for q in self.dsem:
            for i, sem in enumerate(self.dsem[q]):
                if self.dval[q][i] > 0:
                    toks.append((f'd_{q}{i}', sem, self.dval[q][i]))
        return toks

    def barrier(self, engines=None):
        toks = self.all_tokens()
        for e in (engines or self.eng):
            self._wait(e, [t for t in toks if not (t[0] == 'c_' + e)])
        self.last_w = {}
        self.readers = {}

    def mm(self, out, lhsT, rhs, start=True, stop=True):
        return self.op('pe', lambda e: e.matmul(out.ap, lhsT=lhsT.ap, rhs=rhs.ap, start=start, stop=stop),
                       R=[lhsT.key, rhs.key], W=[out.key])

    def tr(self, out, in_, ident):
        return self.op('pe', lambda e: e.transpose(out.ap, in_.ap, ident.ap), R=[in_.key, ident.key], W=[out.key])

    def act(self, out, in_, func, bias=None, scale=None, accum=None, eng='act'):
        kw = {}
        R = [in_.key]
        W = [out.key]
        if bias is not None:
            if isinstance(bias, V):
                kw['bias'] = bias.ap
                R.append(bias.key)
            else:
                kw['bias'] = bias
        if scale is not None:
            if isinstance(scale, V):
                kw['scale'] = scale.ap
                R.append(scale.key)
            else:
                kw['scale'] = scale
        if accum is not None:
            kw['accum_out'] = accum.ap
            W.append(accum.key)
        return self.op('act', lambda e: e.activation(out=out.ap, in_=in_.ap, func=func, **kw), R=R, W=W)

    def tt(self, eng, out, in0, in1, op):
        return self.op(eng, lambda e: e.tensor_tensor(out=out.ap, in0=in0.ap, in1=in1.ap, op=op),
                       R=[in0.key, in1.key], W=[out.key])

    def ts(self, eng, out, in0, s1, op0, s2=None, op1=None, accum=None):
        R = [in0.key]
        W = [out.key]
        a1 = s1
        if isinstance(s1, V):
            a1 = s1.ap
            R.append(s1.key)
        a2 = s2
        if isinstance(s2, V):
            a2 = s2.ap
            R.append(s2.key)
        kw = {}
        if op1 is not None:
            kw['op1'] = op1
        if accum is not None:
            kw['accum_out'] = accum.ap
            W.append(accum.key)
        return self.op(eng, lambda e: e.tensor_scalar(out=out.ap, in0=in0.ap, scalar1=a1, scalar2=a2, op0=op0, **kw),
                       R=R, W=W)

    def stt(self, out, in0, scalar, in1, op0, op1):
        R = [in0.key, in1.key]
        a = scalar
        if isinstance(scalar, V):
            a = scalar.ap
            R.append(scalar.key)
        return self.op('dve', lambda e: e.scalar_tensor_tensor(out=out.ap, in0=in0.ap, scalar=a, in1=in1.ap,
                                                               op0=op0, op1=op1), R=R, W=[out.key])

    def cp(self, eng, out, in_):
        if eng == 'act':
            return self.op('act', lambda e: e.copy(out=out.ap, in_=in_.ap), R=[in_.key], W=[out.key])
        return self.op(eng, lambda e: e.tensor_copy(out=out.ap, in_=in_.ap), R=[in_.key], W=[out.key])

    def red(self, out, in_, op, axis=None):
        ax = axis if axis is not None else AX.X
        return self.op('dve', lambda e: e.tensor_reduce(out=out.ap, in_=in_.ap, axis=ax, op=op),
                       R=[in_.key], W=[out.key])

    def recip(self, out, in_):
        return self.op('dve', lambda e: e.reciprocal(out=out.ap, in_=in_.ap), R=[in_.key], W=[out.key])

    def memset(self, eng, out, val):
        return self.op(eng, lambda e: e.memset(out.ap, val), W=[out.key])

    def ld(self, out, src_ap, src_key=None, q='sp', **kw):
        return self.dma(q, lambda e: e.dma_start(out=out.ap, in_=src_ap, **kw), R=[src_key], W=[out.key])

    def st(self, dst_ap, dst_key, in_, q='sp', **kw):
        return self.dma(q, lambda e: e.dma_start(out=dst_ap, in_=in_.ap, **kw), R=[in_.key], W=[dst_key])


def pipeline(n_items, stages):
    leads = [ld for (_, ld) in stages]
    for t in range(-max(leads), n_items - min(leads)):
        for f, ld in stages:
            i = t + ld
            if 0 <= i < n_items:
                f(i)


class Ring:
    def __init__(self, views):
        self.views = views
        self.i = 0

    def next(self):
        v = self.views[self.i % len(self.views)]
        self.i += 1
        return v


def build(stop=None, dbg=False):
    nc = bass.Bass("TRN2", target_bir_lowering=False)

    def din(name, shape, dt=F32):
        return nc.dram_tensor(name, list(shape), dt, kind="ExternalInput").ap()

    def dscr(name, shape, dt, out=False):
        return nc.dram_tensor(name, list(shape), dt, kind=("ExternalOutput" if out else "Internal")).ap()

    x_in = din("x", [NB, T, D])
    ctx_in = din("ctx", [NB, NCX, D])
    cvec = din("cvec", [3, D])
    ada_w = din("ada_w", [DEPTH, D, 6 * D])
    ada_b = din("ada_b", [DEPTH, 6 * D])
    norm1_g = din("norm1_g", [DEPTH, D])
    w_in = din("w_in", [DEPTH, D, 2560])
    decay_f = din("decay_fwd", [DEPTH, 4])
    decay_b = din("decay_bwd", [DEPTH, 4])
    pool_w = din("pool_w", [DEPTH, 4, 128, 128])
    pool_scale = din("pool_scale", [DEPTH, 512])
    w_out = din("w_out", [DEPTH, D, D])
    norm2_g = din("norm2_g", [DEPTH, D])
    router_w = din("router_w", [DEPTH, D, 36])
    router_b = din("router_b", [DEPTH, 36])
    w_gate = din("exp_w_gate", [DEPTH, NE, D, 512])
    w_up = din("exp_w_up", [DEPTH, NE, D, 512])
    w_down = din("exp_w_down", [DEPTH, NE, 512, D])
    fin_g = din("final_norm_g", [D])
    c_rope_c = din("c_rope_c", [128, 16, 128])
    c_rope_s = din("c_rope_s", [128, 16, 128])
    c_band = din("c_band", [128, 20, 128])
    c_ident = din("c_ident", [128, 128])
    c_utri = din("c_utri", [128, 128])
    c_mtab = din("c_mtab", [128, 4, 128])
    c_pcols = din("c_pcols", [128, 8])
    c_blk = din("c_blk", [128, NBLK])

    out = nc.dram_tensor("out", [NB, T, D], F32, kind="ExternalOutput").ap()
    XS = dscr("XS", [NB, TT, D], F32, out=dbg)
    MOD = dscr("MOD", [DEPTH, 3, 6 * D], F32)
    GXKV = dscr("GXKV", [NB, TT, 2048], BF16)
    SBX = dscr("SBX", [NB, NT, 128, 512], BF16)
    HT = dscr("HT", [NB * TT, D], BF16)
    HB = dscr("HB", [NROWS, D], BF16)
    YB = dscr("YB", [NROWS, D], F32)
    WB2 = dscr("WB", [DEPTH, NE * 128, 12288], BF16)

    with ExitStack() as top:
        S = Sched(nc, top)
        bc_reg = nc.gpsimd.to_reg(DEPTH * NE * 128 - 1)

        uid = [0]

        def sb(st, name, shape, dt):
            uid[0] += 1
            nm = f"{name}_{uid[0]}"
            return V(st.enter_context(nc.sbuf_tensor(nm, list(shape), dt))[:], nm)

        def ps(st, name, shape, dt=F32):
            uid[0] += 1
            nm = f"{name}_{uid[0]}"
            S.excl.add(nm)
            return V(st.enter_context(nc.psum_tensor(nm, list(shape), dt))[:], nm)

        def ring(st, name, n, shape, dt, psum=False):
            f = ps if psum else sb
            return Ring([f(st, f"{name}{i}", shape, dt) for i in range(n)])

        ident_b = sb(top, "ident_b", [128, 128], BF16)
        ident_f = sb(top, "ident_f", [128, 128], F32)
        ones_f = sb(top, "ones_f", [128, 128], F32)
        pcols = sb(top, "pcols", [128, 8], F32)
        epsc = sb(top, "epsc", [128, 1], F32)
        S.ld(ident_f, c_ident[:, :])
        S.ld(ident_b, c_ident[:, :], q='pool')
        S.ld(pcols, c_pcols[:, :])
        S.memset('dve', ones_f, 1.0)
        S.memset('dve', epsc, EPS)

        def rstd_from_ssq(rstd, ssq, tmp, inv_n):
            S.act(tmp, ssq, AF.Sqrt, bias=epsc[0:ssq.ap.shape[0], :], scale=inv_n)
            S.recip(rstd, tmp)

        S.barrier()
        if stop == "C0":
            return nc
        conv_qs = []
        for l_ in range(DEPTH):
            q_ = []
            for e_ in range(NE):
                for part, (wsrc, cc) in enumerate(((w_gate, 8), (w_up, 8), (w_down, 4))):
                    q_.append((wsrc[l_, e_].rearrange("(c p) f -> p c f", p=128),
                               WB2[l_, e_ * 128:(e_ + 1) * 128, part * 4096:(part + 1) * 4096].rearrange("p (c f) -> p c f", c=cc),
                               ('WB', l_, e_, part)))
            conv_qs.append(q_)
        with ExitStack() as st:
            scraw = sb(st, "scraw", [128, 3, 8], F32)
            scT = sb(st, "scT", [128, 8, 32], F32)
            adab = sb(st, "adab", [32, 6 * D], F32)
            wsl = ring(st, "wsl", 4, [128, 8, 512], F32)
            mrow = ring(st, "mrow", 4, [32, 512], F32)
            pm = ring(st, "pm", 4, [128, 512], F32, psum=True)
            for r in range(3):
                S.ld(scraw[:, r, :], cvec[r].rearrange("(p c) -> p c", c=8))
            S.memset('dve', scT, 0.0)
            for r in range(3):
                S.act(scT[:, :, r], scraw[:, r, :], AF.Silu)
            for l in range(DEPTH):
                S.ld(adab, ada_b[l].partition_broadcast(32))
                awv = ada_w[l].rearrange("(p c) n -> p c n", c=8)
                for s in range(12):
                    w = wsl.next()
                    S.ld(w, awv[:, :, s * 512:(s + 1) * 512])
                    if conv_qs[0]:
                        src_, dst_, key_ = conv_qs[0].pop(0)
                        S.dma('pool', lambda en, src_=src_, dst_=dst_: en.dma_start(out=dst_, in_=src_), R=[], W=[key_])
                    p = pm.next()
                    for c in range(8):
                        S.mm(p[0:32, :], scT[:, c, :], w[:, c, :], start=(c == 0), stop=(c == 7))
                    m = mrow.next()
                    S.tt('dve', m, p[0:32, :], adab[:, s * 512:(s + 1) * 512], ALU.add)
                    S.st(MOD[l, :, s * 512:(s + 1) * 512], 'MOD', m[0:3, :])
        S.barrier()

        if stop == "L0":
            return nc

        def x_src(l, b, n):
            if l == 0:
                if n < 2:
                    return ctx_in[b, n * 128:(n + 1) * 128, :], None
                return x_in[b, (n - 2) * 128:(n - 1) * 128, :], None
            return XS[b, n * 128:(n + 1) * 128, :], ('XS', b, n)

        def load_mod_tiles(st, l, rows_segs, gvec, pfx):
            return None

        for l in range(DEPTH):
            last = (l == DEPTH - 1)
            WB = WB2[l]
            conv_q = conv_qs[l]
            conv_next = conv_qs[l + 1] if l + 1 < DEPTH else []

            def conv_issue(k, q=None):
                q = conv_q if q is None else q
                for _ in range(k):
                    if not q:
                        return
                    src_, dst_, key_ = q.pop(0)
                    S.dma('pool', lambda en, src_=src_, dst_=dst_: en.dma_start(out=dst_, in_=src_), R=[], W=[key_])

            with ExitStack() as lst:
                w_in_sb = sb(lst, "w_in_sb", [128, 8, 2560], BF16)
                w_out_sb = sb(lst, "w_out_sb", [128, 8, 1024], BF16)
                rope_c = sb(lst, "rope_c", [128, 16, 128], F32)
                rope_s = sb(lst, "rope_s", [128, 16, 128], F32)
                band = sb(lst, "band", [128, 20, 128], BF16)
                poolw = sb(lst, "poolw", [128, 4, 128], BF16)
                psc = sb(lst, "psc", [128, 4], F32)
                mtab = sb(lst, "mtab", [128, 4, 128], F32)
                maskT = sb(lst, "maskT", [128, 4, 128], F32)
                dtab = sb(lst, "dtab", [128, 6, 4], F32)
                lg = sb(lst, "lg", [128, 2, 4], F32)
                g1t = sb(lst, "g1t", [128, D], F32)
                qT = sb(lst, "qT", [128, 4, TT], BF16)
                kT = sb(lst, "kT", [128, 4, TT], BF16)

                winv = w_in[l].rearrange("(c p) n -> p c n", p=128)
                for c in range(8):
                    for j in range(5):
                        S.ld(w_in_sb[:, c, j * 512:(j + 1) * 512], winv[:, c, j * 512:(j + 1) * 512], q='pool')
                woutv = w_out[l].rearrange("(c p) n -> p c n", p=128)
                for c in range(8):
                    for j in range(2):
                        S.ld(w_out_sb[:, c, j * 512:(j + 1) * 512], woutv[:, c, j * 512:(j + 1) * 512], q='pool')
                S.ld(rope_c, c_rope_c[:, :, :])
                S.ld(rope_s, c_rope_s[:, :, :])
                S.ld(band, c_band[:, :, :], q='pool')
                S.ld(poolw, pool_w[l].rearrange("g c d -> c g d"), q='pool')
                for g in range(4):
                    S.ld(psc[:, g:g + 1], pool_scale[l, g * 128:(g + 1) * 128].rearrange("(p o) -> p o", o=1))
                S.ld(mtab, c_mtab[:, :, :])
                S.ld(g1t, norm1_g[l].partition_broadcast(128))
                S.ld(lg[:, 0, :], decay_f[l].partition_broadcast(128))
                S.ld(lg[:, 1, :], decay_b[l].partition_broadcast(128))
                S.act(lg, lg, AF.Exp, scale=-1.0)
                S.act(lg, lg, AF.Ln, bias=1.0)
                S.ts('dve', lg, lg, -1.0, ALU.mult)
                for ti, (dr, pc) in enumerate([(0, 0), (1, 1), (0, 2), (1, 3), (0, 4), (1, 4)]):
                    S.ts('dve', dtab[:, ti, :], lg[:, dr, :], pcols[:, pc:pc + 1], ALU.mult)
                S.act(dtab, dtab, AF.Exp)
                S.ts('dve', dtab[:, 0:2, :], dtab[:, 0:2, :], KS, ALU.mult)
                with ExitStack() as st:
                    e1 = sb(st, "e1", [128, 128], F32)
                    e2 = sb(st, "e2", [128, 128], F32)
                    for h in range(4):
                        S.act(e1, mtab[:, 0, :], AF.Exp, scale=lg[:, 0, h:h + 1])
                        S.tt('dve', e1, e1, mtab[:, 1, :], ALU.mult)
                        S.act(e2, mtab[:, 2, :], AF.Exp, scale=lg[:, 1, h:h + 1])
                        S.tt('dve', e2, e2, mtab[:, 3, :], ALU.mult)
                        S.tt('dve', e1, e1, e2, ALU.add)
                        S.ts('dve', maskT[:, h, :], e1, KS, ALU.mult)
                S.barrier()
                if stop == f"LS_{l}":
                    return nc

                for b in range(NB):
                    with ExitStack() as st:
                        Am = {}
                        Bm = {}
                        for r in (b, 2):
                            sc = sb(st, f"sc{r}", [128, D], F32)
                            Am[r] = sb(st, f"Am{r}", [128, D], F32)
                            Bm[r] = sb(st, f"Bm{r}", [128, D], F32)
                            S.ld(sc, MOD[l, r, 1 * D:2 * D].partition_broadcast(128), 'MOD')
                            S.ld(Bm[r], MOD[l, r, 0:D].partition_broadcast(128), 'MOD')
                            S.stt(Am[r], sc, 1.0, g1t, ALU.add, ALU.mult)
                        xt_r = ring(st, "xt", 2, [128, D], F32)
                        junk = sb(st, "junk", [128, D], BF16)
                        ssq_r = ring(st, "ssq", 2, [128, 1], F32)
                        tmp1_r = ring(st, "tmp1", 2, [128, 1], F32)
                        rstd_r = ring(st, "rstd", 2, [128, 1], F32)
                        t32_r = ring(st, "t32", 2, [128, D], F32)
                        hb_r = ring(st, "hb", 2, [128, D], BF16)
                        hT_r = ring(st, "hT", 2, [128, 8, 128], BF16)
                        r1 = sb(st, "r1", [128, 512], F32)
                        r2 = sb(st, "r2", [128, 512], F32)
                        qtm_r = ring(st, "qtm", 2, [128, 512], BF16)
                        stage_r = ring(st, "stage", 2, [128, 2048], BF16)
                        pT = ps(st, "pT", [128, 1024], BF16)
                        pP = [ps(st, f"pP{j}", [128, 512], F32) for j in range(5)]
                        pQK = ps(st, "pQK", [128, 1024], BF16)
                        cx = {}

                        def a0(n):
                            c_ = cx[n] = {}
                            conv_issue(2)
                            src, skey = x_src(l, b, n)
                            c_['xt'] = xt_r.next()
                            S.ld(c_['xt'], src, skey)

                        def a1(n):
                            c_ = cx[n]
                            r = 2 if n < 2 else b
                            xt = c_['xt']
                            ssq = ssq_r.next(); tmp1 = tmp1_r.next(); rstd = rstd_r.next()
                            t32 = t32_r.next()
                            c_['hb'] = hb_r.next()
                            S.act(junk, xt, AF.Square, accum=ssq)
                            rstd_from_ssq(rstd, ssq, tmp1, 1.0 / D)
                            S.stt(t32, xt, rstd, Am[r], ALU.mult, ALU.mult)
                            S.tt('pool', c_['hb'], t32, Bm[r], ALU.add)

                        def a2(n):
                            c_ = cx[n]
                            hb = c_['hb']
                            for c in range(8):
                                S.tr(pT[:, c * 128:(c + 1) * 128], hb[:, c * 128:(c + 1) * 128], ident_b)
                            c_['hT'] = hT_r.next()
                            S.cp('act', c_['hT'], pT.re("p (c t) -> p c t", c=8))

                        def a3(n):
                            c_ = cx[n]
                            hT = c_['hT']
                            for j in range(5):
                                for c in range(8):
                                    S.mm(pP[j], hT[:, c, :], w_in_sb[:, c, j * 512:(j + 1) * 512],
                                         start=(c == 0), stop=(c == 7))

                        def a4(n):
                            c_ = cx[n]
                            isctx = n < 2
                            stage = c_['stage'] = stage_r.next()
                            qtm = c_['qtm'] = qtm_r.next()
                            if isctx:
                                S.cp('act', qtm, pP[0])
                                S.cp('dve', stage[:, 0:512], pP[1])
                            else:
                                tn = n - 2
                                cb = rope_c[:, tn, :].bc(1, [128, 4, 128])
                                for (src_p, dst) in ((pP[0], qtm), (pP[1], stage[:, 0:512])):
                                    p3 = src_p.re("p (h d) -> p h d", h=4)
                                    p5 = src_p.re("p (h f a d) -> p h f a d", h=4, f=2, a=2)
                                    r25 = r2.re("p (h f a d) -> p h f a d", h=4, f=2, a=2)
                                    s5 = rope_s[:, tn, :].re("p (f a d) -> p f a d", f=2, a=2)
                                    S.tt('dve', r1.re("p (h d) -> p h d", h=4), p3, cb, ALU.mult)
                                    for a in range(2):
                                        S.tt('dve', r25[:, :, :, a, :], p5[:, :, :, 1 - a, :],
                                             s5[:, :, a, :].bc(1, [128, 4, 2, 32]), ALU.mult)
                                    S.tt('pool', dst, r1, r2, ALU.add)
                            S.cp('act', stage[:, 512:1024], pP[2])
                            S.act(stage[:, 1024:1536], pP[3], AF.Silu)
                            S.cp('act', stage[:, 1536:2048], pP[4])
                            S.st(GXKV[b, n * 128:(n + 1) * 128, :], ('GXKV', b, n), stage)

                        def a5(n):
                            c_ = cx[n]
                            qtm = c_['qtm']; stage = c_['stage']
                            for h in range(4):
                                S.tr(pQK[:, h * 128:(h + 1) * 128], qtm[:, h * 128:(h + 1) * 128], ident_b)
                            for h in range(4):
                                S.tr(pQK[:, 512 + h * 128:512 + (h + 1) * 128], stage[:, h * 128:(h + 1) * 128], ident_b)
                            S.cp('act', qT.sub((slice(None), slice(None), slice(n * 128, (n + 1) * 128)), ('qT', n)),
                                 pQK[:, 0:512].re("p (h t) -> p h t", h=4))
                            S.cp('dve', kT.sub((slice(None), slice(None), slice(n * 128, (n + 1) * 128)), ('kT', n)),
                                 pQK[:, 512:1024].re("p (h t) -> p h t", h=4))
                            del cx[n]

                        pipeline(NT, [(a0, 3), (a1, 2), (a2, 1), (a3, 0), (a4, 0), (a5, -1)])
                    S.barrier()
                    if stop == f"M1_{l}_{b}":
                        return nc

                    with ExitStack() as st:
                        S32 = sb(st, "S32", [128, 512], F32)
                        sbf_r = ring(st, "sbf", 2, [128, 512], BF16)
                        kv_r = ring(st, "kv", 4, [128, 1024], BF16)
                        vb_r = ring(st, "vb", 2, [128, 512], BF16)
                        pU_r = ring(st, "pU", 2, [128, 512], F32, psum=True)
                        S.memset('dve', S32, 0.0)
                        order = [1, 0] + list(range(NT - 1, 1, -1))
                        cx = {}

                        def b0(i):
                            n = order[i]
                            c_ = cx[i] = {}
                            c_['kv'] = kv_r.next()
                            S.ld(c_['kv'], GXKV[b, n * 128:(n + 1) * 128, 0:1024], ('GXKV', b, n))

                        def b1a(i):
                            c_ = cx[i]
                            kv = c_['kv']
                            vb = c_['vb'] = vb_r.next()
                            S.tt('pool', vb.re("p (h e) -> p h e", h=4), kv[:, 512:1024].re("p (h e) -> p h e", h=4),
                                 dtab[:, 3, :].bc(2, [128, 4, 128]), ALU.mult)

                        def b1(i):
                            c_ = cx[i]
                            kv = c_['kv']
                            vb = c_['vb']
                            pU = c_['pU'] = pU_r.next()
                            for h in range(4):
                                S.mm(pU[:, h * 128:(h + 1) * 128], kv[:, h * 128:(h + 1) * 128],
                                     vb[:, h * 128:(h + 1) * 128])

                        def b2(i):
                            n = order[i]
                            c_ = cx[i]
                            sbf = sbf_r.next()
                            S.cp('act', sbf, S32)
                            S.st(SBX[b, n, :, :], ('SBX', b, n), sbf)
                            S.tt('dve', S32.re("p (h e) -> p h e", h=4), S32.re("p (h e) -> p h e", h=4),
                                 dtab[:, 5, :].bc(2, [128, 4, 128]), ALU.mult)
                            S.tt('dve', S32, S32, c_['pU'], ALU.add)
                            del cx[i]

                        pipeline(NT, [(b0, 3), (b1a, 2), (b1, 1), (b2, 0)])
                    S.barrier()

                    with ExitStack() as st:
                        G1 = {}
                        for r in (b, 2):
                            G1[r] = sb(st, f"G1{r}", [128, D], F32)
                            S.ld(G1[r], MOD[l, r, 2 * D:3 * D].partition_broadcast(128), 'MOD')
                        S32 = sb(st, "S32f", [128, 512], F32)
                        sfb_r = ring(st, "sfb", 2, [128, 512], BF16)
                        gx_r = ring(st, "gx", 3, [128, 2048], BF16)
                        sbx_r = ring(st, "sbx", 3, [128, 512], BF16)
                        xpp_r = ring(st, "xpp", 2, [128, 512], BF16)
                        xpn_r = ring(st, "xpn", 2, [128, 512], BF16)
                        xt_r = ring(st, "xt3", 3, [128, D], F32)
                        pm_r = ring(st, "pmk", 2, [128, 512], BF16)
                        o32 = sb(st, "o32", [128, 512], F32)
                        u32 = sb(st, "u32", [128, 512], F32)
                        sq32 = sb(st, "sq32", [128, 512], F32)
                        ssqh = sb(st, "ssqh", [128, 4], F32)
                        tmph = sb(st, "tmph", [128, 4], F32)
                        rsh = sb(st, "rsh", [128, 4], F32)
                        ret_r = ring(st, "ret", 3, [128, 512], BF16)
                        mT_r = ring(st, "mT", 4, [128, 8, 128], BF16)
                        dT = sb(st, "dT", [128, 4, 128], BF16)
                        yg = sb(st, "yg", [128, D], F32)
                        xn_r = ring(st, "xn", 2, [128, D], F32)
                        vf_r = ring(st, "vf", 2, [128, 512], BF16)
                        pS = ps(st, "pS", [128, 512], F32)
                        pA = ps(st, "pA", [128, 512], F32)
                        pB = ps(st, "pB", [128, 512], F32)
                        pC = ps(st, "pC", [128, 512], F32)
                        pR = ps(st, "pR", [128, 1024], BF16)
                        pD = ps(st, "pD", [128, 512], F32)
                        pY = [ps(st, f"pY{j}", [128, 512], F32) for j in range(2)]
                        S.memset('dve', S32, 0.0)
                        sfb0 = sfb_r.next()
                        S.cp('act', sfb0, S32)
                        cx = {}
                        sfb_cur = {0: sfb0}

                        def c0(n):
                            c_ = cx[n] = {}
                            conv_issue(2)
                            isctx = n < 2
                            c_['do_out'] = not (last and isctx)
                            c_['first'] = n in (0, 2)
                            c_['lastt'] = n in (1, NT - 1)
                            gx = c_['gx'] = gx_r.next()
                            S.ld(gx, GXKV[b, n * 128:(n + 1) * 128, :], ('GXKV', b, n))
                            if c_['do_out']:
                                c_['sbx'] = sbx_r.next()
                                S.ld(c_['sbx'], SBX[b, n, :, :], ('SBX', b, n))
                                c_['xpp'] = c_['xpn'] = None
                                if not c_['first']:
                                    c_['xpp'] = xpp_r.next()
                                    S.ld(c_['xpp'], GXKV[b, (n - 1) * 128:n * 128, 1536:2048], ('GXKV', b, n - 1))
                                if not c_['lastt']:
                                    c_['xpn'] = xpn_r.next()
                                    S.ld(c_['xpn'], GXKV[b, (n + 1) * 128:(n + 2) * 128, 1536:2048], ('GXKV', b, n + 1))

                        def c0b(n):
                            c_ = cx[n]
                            if c_['do_out']:
                                c_['xt'] = xt_r.next()
                                src, skey = x_src(l, b, n)
                                S.ld(c_['xt'], src, skey)

                        def c1(n):
                            c_ = cx[n]
                            gx = c_['gx']
                            vf = c_['vf'] = vf_r.next()
                            S.tt('pool', vf.re("p (h e) -> p h e", h=4), gx[:, 512:1024].re("p (h e) -> p h e", h=4),
                                 dtab[:, 2, :].bc(2, [128, 4, 128]), ALU.mult)
                            if not c_['do_out']:
                                return
                            qTn = qT.sub((slice(None), slice(None), slice(n * 128, (n + 1) * 128)), ('qT', n))
                            kTn = kT.sub((slice(None), slice(None), slice(n * 128, (n + 1) * 128)), ('kT', n))
                            for h in range(4):
                                S.mm(pS[:, h * 128:(h + 1) * 128], kTn[:, h, :], qTn[:, h, :])
                            pmk = c_['pmk'] = pm_r.next()
                            S.tt('dve', pmk, pS, maskT.re("p h i -> p (h i)"), ALU.mult)
                            mT = c_['mT'] = mT_r.next()
                            xpp = c_['xpp']; xpn = c_['xpn']
                            for g in range(4):
                                terms = []
                                if xpp is not None:
                                    terms.append((xpp[:, g * 128:(g + 1) * 128], band[:, g * 5 + 0, :]))
                                kind = 3 if c_['first'] else (4 if c_['lastt'] else 1)
                                terms.append((gx[:, 1536 + g * 128:1536 + (g + 1) * 128], band[:, g * 5 + kind, :]))
                                if xpn is not None:
                                    terms.append((xpn[:, g * 128:(g + 1) * 128], band[:, g * 5 + 2, :]))
                                for ti, (a_, b_) in enumerate(terms):
                                    S.mm(pD[:, g * 128:(g + 1) * 128], a_, b_, start=(ti == 0),
                                         stop=(ti == len(terms) - 1))
                            S.cp('act', dT, pD.re("p (g t) -> p g t", g=4))

                        def c1b(n):
                            c_ = cx[n]
                            if not c_['do_out']:
                                return
                            mT = c_['mT']
                            for g in range(4):
                                S.mm(pD[:, g * 128:(g + 1) * 128], poolw[:, g, :], dT[:, g, :])
                            S.tt('dve', mT[:, 4:8, :], pD.re("p (g t) -> p g t", g=4),
                                 psc.bc(2, [128, 4, 128]), ALU.mult)

                        def c2(n):
                            c_ = cx[n]
                            gx = c_['gx']
                            ksl = gx[:, 0:512]
                            vsl = gx[:, 512:1024]
                            sfb = sfb_cur[n]
                            if c_['do_out']:
                                qTn = qT.sub((slice(None), slice(None), slice(n * 128, (n + 1) * 128)), ('qT', n))
                                pmk = c_['pmk']; sbx = c_['sbx']
                                for h in range(4):
                                    S.mm(pA[:, h * 128:(h + 1) * 128], pmk[:, h * 128:(h + 1) * 128],
                                         vsl[:, h * 128:(h + 1) * 128])
                                for h in range(4):
                                    S.mm(pB[:, h * 128:(h + 1) * 128], qTn[:, h, :], sfb[:, h * 128:(h + 1) * 128])
                                for h in range(4):
                                    S.mm(pC[:, h * 128:(h + 1) * 128], qTn[:, h, :], sbx[:, h * 128:(h + 1) * 128])
                            vf = c_['vf']
                            for h in range(4):
                                S.mm(pS[:, h * 128:(h + 1) * 128], ksl[:, h * 128:(h + 1) * 128],
                                     vf[:, h * 128:(h + 1) * 128])
                            S.tt('dve', S32.re("p (h e) -> p h e", h=4), S32.re("p (h e) -> p h e", h=4),
                                 dtab[:, 4, :].bc(2, [128, 4, 128]), ALU.mult)
                            S.tt('dve', S32, S32, pS, ALU.add)
                            sfbn = sfb_r.next()
                            S.cp('act', sfbn, S32)
                            sfb_cur[n + 1] = sfbn
                            if c_['do_out']:
                                S.tt('dve', o32.re("p (h e) -> p h e", h=4), pB.re("p (h e) -> p h e", h=4),
                                     dtab[:, 0, :].bc(2, [128, 4, 128]), ALU.mult)
                                S.tt('dve', u32.re("p (h e) -> p h e", h=4), pC.re("p (h e) -> p h e", h=4),
                                     dtab[:, 1, :].bc(2, [128, 4, 128]), ALU.mult)
                                S.tt('pool', o32, o32, u32, ALU.add)
                                S.tt('dve', o32, o32, pA, ALU.add)
                                S.act(sq32, o32, AF.Square)
                                S.red(ssqh, sq32.re("p (h e) -> p h e", h=4), ALU.add)
                                rstd_from_ssq(rsh, ssqh, tmph, 1.0 / 128)
                                S.tt('pool', u32.re("p (h e) -> p h e", h=4), o32.re("p (h e) -> p h e", h=4),
                                     rsh.bc(2, [128, 4, 128]), ALU.mult)
                                ret = c_['ret'] = ret_r.next()
                                S.tt('pool', ret, u32, gx[:, 1024:1536], ALU.mult)

                        def c3(n):
                            c_ = cx[n]
                            if c_['do_out']:
                                r = 2 if n < 2 else b
                                ret = c_['ret']; mT = c_['mT']; xt = c_['xt']
                                for h in range(4):
                                    S.tr(pR[:, h * 128:(h + 1) * 128], ret[:, h * 128:(h + 1) * 128], ident_b)
                                S.cp('act', mT[:, 0:4, :], pR[:, 0:512].re("p (h t) -> p h t", h=4))

                        def c3b(n):
                            c_ = cx[n]
                            if c_['do_out']:
                                r = 2 if n < 2 else b
                                mT = c_['mT']; xt = c_['xt']
                                for j in range(2):
                                    for c in range(8):
                                        S.mm(pY[j], mT[:, c, :], w_out_sb[:, c, j * 512:(j + 1) * 512],
                                             start=(c == 0), stop=(c == 7))
                                for j in range(2):
                                    S.tt('dve', yg[:, j * 512:(j + 1) * 512], pY[j], G1[r][:, j * 512:(j + 1) * 512],
                                         ALU.mult)
                                xn = xn_r.next()
                                S.tt('pool', xn, xt, yg, ALU.add)
                                S.st(XS[b, n * 128:(n + 1) * 128, :], ('XS', b, n), xn)
                            del cx[n]

                        pipeline(NT, [(c0, 2), (c1, 1), (c3, -2), (c0b, 0), (c2, 0), (c1b, 1), (c3b, -2)])
                    S.barrier()
                    if stop == f"M3_{l}_{b}":
                        return nc
            S.barrier()
            if stop == f"MIX_{l}":
                return nc

            conv_issue(len(conv_q))
            S.barrier()
            tiles = [(b, n) for b in range(NB) for n in range(NT) if not (last and n < 2)]
            NG = len(tiles)
            with ExitStack() as lst:
                OH1 = sb(lst, "OH1", [128, NG, 32], F32)
                OH2 = sb(lst, "OH2", [128, NG, 32], F32)
                W12 = sb(lst, "W12", [128, NG, 2], F32)
                RK = sb(lst, "RK", [128, NG, 2], F32)
                DEST = sb(lst, "DEST", [128, NG, 2], I32)
                widx = sb(lst, "widx", [128, NBLK], I32)
                g2t = sb(lst, "g2t", [128, D], F32)
                pstart = sb(lst, "pstart", [128, 32], F32)
                S.ld(g2t, norm2_g[l].partition_broadcast(128))
                rows = sorted(set(2 if n < 2 else b for (b, n) in tiles))
                with ExitStack() as st:
                    Am = {}
                    Bm = {}
                    for r in rows:
                        sc = sb(st, f"sc2{r}", [128, D], F32)
                        Am[r] = sb(st, f"A2{r}", [128, D], F32)
                        Bm[r] = sb(st, f"B2{r}", [128, D], F32)
                        S.ld(sc, MOD[l, r, 4 * D:5 * D].partition_broadcast(128), 'MOD')
                        S.ld(Bm[r], MOD[l, r, 3 * D:4 * D].partition_broadcast(128), 'MOD')
                        S.stt(Am[r], sc, 1.0, g2t, ALU.add, ALU.mult)
                    wr = sb(st, "wr", [128, 8, 36], F32)
                    rbias = sb(st, "rbias", [128, 36], F32)
                    utri = sb(st, "utri", [128, 128], F32)
                    Racc = sb(st, "Racc", [128, 32], F32)
                    zt = sb(st, "zt", [128, 4096], BF16)
                    S.memset('pool', zt, 0.0)
                    HBv = HB.rearrange("(p a) d -> p (a d)", p=128)
                    for kz in range(NBLK * D // 4096):
                        S.st(HBv[:, kz * 4096:(kz + 1) * 4096], 'HB', zt)
                    S.ld(wr, router_w[l].rearrange("(c p) n -> p c n", p=128))
                    S.ld(rbias, router_b[l].partition_broadcast(128))
                    S.ld(utri, c_utri[:, :])
                    S.memset('dve', Racc, 0.0)
                    xt_r = ring(st, "xtr", 2, [128, D], F32)
                    junk = sb(st, "junkr", [128, D], BF16)
                    ssq = sb(st, "ssqr", [128, 1], F32)
                    tmp1 = sb(st, "tmp1r", [128, 1], F32)
                    rstd = sb(st, "rstdr", [128, 1], F32)
                    t32 = sb(st, "t32r", [128, D], F32)
                    h32_r = ring(st, "h32", 2, [128, D], F32)
                    hbp_r = ring(st, "hbp", 2, [128, D], BF16)
                    h32T = sb(st, "h32T", [128, 8, 128], F32)
                    lgt = sb(st, "lgt", [128, 36], F32)
                    sm = sb(st, "sm", [128, 16], F32)
                    ohg = sb(st, "ohg", [128, 4], F32)
                    j4 = sb(st, "j4", [128, 4], F32)
                    sel = sb(st, "sel", [128, 4, 8], F32)
                    ein = sb(st, "ein", [128, 8], F32)
                    m8 = sb(st, "m8", [128, 8], F32)
                    eq1 = sb(st, "eq1", [128, 8], F32)
                    eq2 = sb(st, "eq2", [128, 8], F32)
                    OHs = sb(st, "OHs", [128, 32], F32)
                    t32b = sb(st, "t32b", [128, 32], F32)
                    pTr = [ps(st, f"pTr{j}", [128, 512], F32) for j in range(2)]
                    pL = ps(st, "pL", [128, 512], F32)
                    pCn = ps(st, "pCn", [128, 512], F32)
                    cx = {}

                    def r0(gt):
                        b, n = tiles[gt]
                        c_ = cx[gt] = {}
                        conv_issue(2, conv_next)
                        c_['xt'] = xt_r.next()
                        S.ld(c_['xt'], XS[b, n * 128:(n + 1) * 128, :], ('XS', b, n))

                    def r1(gt):
                        b, n = tiles[gt]
                        c_ = cx[gt]
                        r = 2 if n < 2 else b
                        xt = c_['xt']
                        S.act(junk, xt, AF.Square, accum=ssq)
                        S.act(tmp1, ssq, AF.Ln, bias=epsc, scale=1.0 / D)
                        S.act(rstd, tmp1, AF.Exp, scale=-0.5)
                        S.stt(t32, xt, rstd, Am[r], ALU.mult, ALU.mult)
                        h32 = c_['h32'] = h32_r.next()
                        S.tt('pool', h32, t32, Bm[r], ALU.add)
                        hbp = hbp_r.next()
                        S.cp('act', hbp, h32)
                        S.st(HT[gt * 128:(gt + 1) * 128, :], ('HT', gt), hbp)

                    def r2(gt):
                        c_ = cx[gt]
                        h32 = c_['h32']
                        for c in range(8):
                            S.tr(pTr[c // 4][:, (c % 4) * 128:(c % 4 + 1) * 128], h32[:, c * 128:(c + 1) * 128], ident_f)
                        S.cp('dve', h32T[:, 0:4, :], pTr[0].re("p (c t) -> p c t", c=4))
                        S.cp('act', h32T[:, 4:8, :], pTr[1].re("p (c t) -> p c t", c=4))
                        for c in range(8):
                            S.mm(pL[:, 0:36], h32T[:, c, :], wr[:, c, :], start=(c == 0), stop=(c == 7))
                        S.tt('dve', lgt, pL[:, 0:36], rbias, ALU.add)
                        gl = lgt[:, 0:4]
                        S.red(sm[:, 0:1], gl, ALU.max)
                        S.ts('dve', ohg, gl, sm[:, 0:1], ALU.is_equal)
                        S.ts('dve', sm[:, 1:2], sm[:, 0:1], -1.0, ALU.mult)
                        S.act(j4, gl, AF.Exp, bias=sm[:, 1:2], accum=sm[:, 2:3])
                        S.recip(sm[:, 3:4], sm[:, 2:3])
                        S.tt('dve', sel, lgt[:, 4:36].re("p (g e) -> p g e", g=4), ohg.bc(2, [128, 4, 8]), ALU.mult)
                        S.red(ein, sel.re("p g e -> p e g"), ALU.add)
                        S.op('dve', lambda e: e.max(out=m8.ap, in_=ein.ap), R=[ein.key], W=[m8.key])
                        S.ts('dve', eq1, ein, m8[:, 0:1], ALU.is_equal)
                        S.ts('dve', eq2, ein, m8[:, 1:2], ALU.is_equal)
                        S.tt('dve', sm[:, 4:5], m8[:, 1:2], m8[:, 0:1], ALU.subtract)
                        S.act(sm[:, 5:6], sm[:, 4:5], AF.Exp)
                        S.ts('dve', sm[:, 6:7], sm[:, 5:6], 1.0, ALU.add)
                        S.recip(sm[:, 7:8], sm[:, 6:7])
                        w12 = W12.sub((slice(None), gt, slice(None)), ('W12', gt))
                        S.tt('dve', w12[:, 0:1], sm[:, 7:8], sm[:, 3:4], ALU.mult)
                        S.tt('dve', w12[:, 1:2], w12[:, 0:1], sm[:, 5:6], ALU.mult)
                        oh1 = OH1.sub((slice(None), gt, slice(None)), ('OH1', gt))
                        oh2 = OH2.sub((slice(None), gt, slice(None)), ('OH2', gt))
                        S.tt('dve', oh1.re("p (g e) -> p g e", g=4), ohg.bc(2, [128, 4, 8]), eq1.bc(1, [128, 4, 8]), ALU.mult)
                        S.tt('dve', oh2.re("p (g e) -> p g e", g=4), ohg.bc(2, [128, 4, 8]), eq2.bc(1, [128, 4, 8]), ALU.mult)
                        S.tt('dve', OHs, oh1, oh2, ALU.add)
                        S.mm(pCn[:, 0:32], utri, OHs, start=True, stop=False)
                        S.mm(pCn[:, 0:32], ones_f, Racc, start=False, stop=True)
                        rk = RK.sub((slice(None), gt, slice(None)), ('RK', gt))
                        S.tt('dve', t32b, oh1, pCn[:, 0:32], ALU.mult)
                        S.red(rk[:, 0:1], t32b, ALU.add)
                        S.tt('dve', t32b, oh2, pCn[:, 0:32], ALU.mult)
                        S.red(rk[:, 1:2], t32b, ALU.add)
                        S.tt('dve', Racc, Racc, OHs, ALU.add)
                        del cx[gt]

                    pipeline(NG, [(r0, 2), (r1, 1), (r2, 0)])
                    cnt = sb(st, "cnt", [128, 32], F32)
                    cnti = sb(st, "cnti", [128, 32], I32)
                    padded = sb(st, "padded", [128, 32], F32)
                    ca = sb(st, "ca", [128, 32], F32)
                    cb_ = sb(st, "cb_", [128, 32], F32)
                    cmp = sb(st, "cmp", [128, NBLK, 32], F32)
                    bef = sb(st, "bef", [128, NBLK], F32)
                    same = sb(st, "same", [128, NBLK], F32)
                    blk = sb(st, "blk", [128, NBLK], F32)
                    S.ld(blk, c_blk[:, :])
                    S.mm(pCn[:, 0:32], ones_f, Racc)
                    S.cp('dve', cnt, pCn[:, 0:32])
                    S.ts('dve', cnti, cnt, 127.0, ALU.add)
                    S.ts('dve', cnti, cnti, 7, ALU.arith_shift_right, s2=7, op1=ALU.logical_shift_left)
                    S.cp('dve', padded, cnti)
                    cur, oth = ca, cb_
                    S.cp('dve', cur, padded)
                    for s in (1, 2, 4, 8, 16):
                        S.cp('dve', oth, cur)
                        S.tt('dve', oth[:, s:32], cur[:, s:32], cur[:, 0:32 - s], ALU.add)
                        cur, oth = oth, cur
                    pends = cur
                    S.tt('dve', pstart, pends, padded, ALU.subtract)
                    S.tt('dve', cmp, pends.bc(1, [128, NBLK, 32]), blk.bc(2, [128, NBLK, 32]), ALU.is_le)
                    S.red(bef, cmp, ALU.add)
                    S.ts('dve', bef, bef, 31.0, ALU.min)
                    S.ts('dve', bef, bef, 128.0, ALU.mult, s2=pcols[:, 3:4], op1=ALU.add)
                    S.memset('dve', same, 0.0)
                    S.tt('dve', same[:, 1:NBLK], bef[:, 1:NBLK], bef[:, 0:NBLK - 1], ALU.is_equal)
                    for qq in range(1, NSTREAM):
                        S.memset('dve', same[:, qq * BPS:qq * BPS + 1], 0.0)
                    S.ts('dve', bef, bef, float(l * NE * 128), ALU.add)
                    S.stt(bef, same, float(BIGIDX), bef, ALU.mult, ALU.add)
                    S.cp('dve', widx, bef)
                    hb2_r = ring(st, "hb2", 2, [128, D], BF16)
                    dsf = sb(st, "dsf", [128, 2], F32)
                    for gt, (b, n) in enumerate(tiles):
                        rk = RK.sub((slice(None), gt, slice(None)), ('RK', gt))
                        dst = DEST.sub((slice(None), gt, slice(None)), ('DEST', gt))
                        for k, OHk in enumerate((OH1, OH2)):
                            ohk = OHk.sub((slice(None), gt, slice(None)), (OHk.key, gt))
                            S.tt('dve', t32b, ohk, pstart, ALU.mult)
                            S.red(dsf[:, k:k + 1], t32b, ALU.add)
                        S.tt('dve', dsf, dsf, rk, ALU.add)
                        S.cp('dve', dst, dsf)
                        hb2 = hb2_r.next()
                        S.ld(hb2, HT[gt * 128:(gt + 1) * 128, :], ('HT', gt))
                        for k in range(2):
                            S.dma('pool', lambda e, k=k, dst=dst, hb2=hb2: e.indirect_dma_start(
                                out=HB[:, :], out_offset=bass.IndirectOffsetOnAxis(ap=dst.ap[:, k:k + 1], axis=0),
                                in_=hb2.ap, in_offset=None), R=[hb2.key, dst.key], W=['HB'])
                S.barrier()
                if stop == f"R_{l}":
                    return nc

                with ExitStack() as st:
                    wb_r = ring(st, "wbuf", NSTREAM, [128, 12288], BF16)
                    hg_r = ring(st, "hg", 5, [128, D], BF16)
                    hgT_r = ring(st, "hgT", 2, [128, 8, 128], BF16)
                    sg_r = ring(st, "sg", 2, [128, 512], F32)
                    actb_r = ring(st, "actb", 2, [128, 512], BF16)
                    actT_r = ring(st, "actT", 2, [128, 4, 128], BF16)
                    yb_r = ring(st, "yb", 2, [128, D], F32)
                    pT = ps(st, "pTe", [128, 1024], BF16)
                    pG_r = ring(st, "pG", 2, [128, 512], F32, psum=True)
                    pUp_r = ring(st, "pUp", 2, [128, 512], F32, psum=True)
                    pT2 = ps(st, "pT2", [128, 1024], BF16)
                    pY = [ps(st, f"pYe{j}", [128, 512], F32) for j in range(2)]
                    cx = {}

                    def e_s0(j):
                        i = (j % NSTREAM) * BPS + j // NSTREAM
                        c_ = cx[j] = {'i': i}
                        wb_ = wb_r.next()
                        S.dma('pool', lambda e, wb_=wb_, i=i: e.indirect_dma_start(
                            out=wb_.ap, out_offset=None, in_=WB2.rearrange("l r x -> (l r) x"),
                            in_offset=bass.IndirectOffsetOnAxis(ap=widx.ap[:, i:i + 1], axis=0),
                            bounds_check=bc_reg, oob_is_err=False),
                            R=[widx.key], W=[wb_.key])
                        c_['wg3'] = wb_[:, 0:4096].re("p (c f) -> p c f", c=8)
                        c_['wu3'] = wb_[:, 4096:8192].re("p (c f) -> p c f", c=8)
                        c_['wd'] = wb_[:, 8192:12288].re("p (c f) -> p c f", c=4)
                        c_['hg'] = hg_r.next()
                        S.ld(c_['hg'], HB[i * 128:(i + 1) * 128, :], 'HB')

                    def e_s1(j):
                        c_ = cx[j]
                        hg = c_['hg']
                        for c in range(8):
                            S.tr(pT[:, c * 128:(c + 1) * 128], hg[:, c * 128:(c + 1) * 128], ident_b)
                        c_['hgT'] = hgT_r.next()
                        S.cp('act', c_['hgT'], pT.re("p (c t) -> p c t", c=8))

                    def e_s2(j):
                        c_ = cx[j]
                        c_['pG'] = pG_r.next()
                        c_['pUp'] = pUp_r.next()
                        for c in range(8):
                            S.mm(c_['pG'], c_['hgT'][:, c, :], c_['wg3'][:, c, :], start=(c == 0), stop=(c == 7))
                        for c in range(8):
                            S.mm(c_['pUp'], c_['hgT'][:, c, :], c_['wu3'][:, c, :], start=(c == 0), stop=(c == 7))

                    def e_s3(j):
                        c_ = cx[j]
                        sg = sg_r.next()
                        c_['actb'] = actb_r.next()
                        S.act(sg, c_['pG'], AF.Silu)
                        S.tt('dve', c_['actb'], sg, c_['pUp'], ALU.mult)

                    def e_s4(j):
                        c_ = cx[j]
                        for c in range(4):
                            S.tr(pT2[:, c * 128:(c + 1) * 128], c_['actb'][:, c * 128:(c + 1) * 128], ident_b)
                        c_['actT'] = actT_r.next()
                        S.cp('act', c_['actT'], pT2[:, 0:512].re("p (c t) -> p c t", c=4))

                    def e_s5(j):
                        c_ = cx[j]
                        for jj in range(2):
                            for c in range(4):
                                S.mm(pY[jj], c_['actT'][:, c, :], c_['wd'][:, c, jj * 512:(jj + 1) * 512],
                                     start=(c == 0), stop=(c == 3))
                        yb = yb_r.next()
                        S.cp('dve', yb[:, 0:512], pY[0])
                        S.cp('act', yb[:, 512:1024], pY[1])
                        i = c_['i']
                        S.st(YB[i * 128:(i + 1) * 128, :], 'YB', yb)
                        del cx[j]

                    pipeline(NBLK, [(e_s0, 4), (e_s4, -1), (e_s1, 1), (e_s2, 0), (e_s3, 0), (e_s5, -1)])
                S.barrier()
                if stop == f"E_{l}":
                    return nc

                with ExitStack() as st:
                    G2 = {}
                    for r in rows:
                        G2[r] = sb(st, f"G2{r}", [128, D], F32)
                        S.ld(G2[r], MOD[l, r, 5 * D:6 * D].partition_broadcast(128), 'MOD')
                    fg = sb(st, "fg", [128, D], F32)
                    S.ld(fg, fin_g.partition_broadcast(128))
                    y1_r = ring(st, "y1", 3, [128, D], F32)
                    y2_r = ring(st, "y2", 3, [128, D], F32)
                    xt_r = ring(st, "xtf", 3, [128, D], F32)
                    ya = sb(st, "ya", [128, D], F32)
                    xn_r = ring(st, "xnf", 2, [128, D], F32)
                    junk = sb(st, "junkf", [128, D], BF16)
                    ssq = sb(st, "ssqf", [128, 1], F32)
                    tmp1 = sb(st, "tmp1f", [128, 1], F32)
                    rstd = sb(st, "rstdf", [128, 1], F32)
                    ot_r = ring(st, "ot", 2, [128, D], F32)
                    cx = {}

                    def f0(gt):
                        b, n = tiles[gt]
                        c_ = cx[gt] = {}
                        dst = DEST.sub((slice(None), gt, slice(None)), ('DEST', gt))
                        c_['y1'] = y1_r.next()
                        c_['y2'] = y2_r.next()
                        for k, yk in enumerate((c_['y1'], c_['y2'])):
                            S.dma('pool', lambda e, k=k, yk=yk, dst=dst: e.indirect_dma_start(
                                out=yk.ap, out_offset=None, in_=YB[:, :],
                                in_offset=bass.IndirectOffsetOnAxis(ap=dst.ap[:, k:k + 1], axis=0)),
                                R=['YB', dst.key], W=[yk.key])
                        c_['xt'] = xt_r.next()
                        S.ld(c_['xt'], XS[b, n * 128:(n + 1) * 128, :], ('XS', b, n))

                    def f1(gt):
                        b, n = tiles[gt]
                        c_ = cx[gt]
                        r = 2 if n < 2 else b
                        w12 = W12.sub((slice(None), gt, slice(None)), ('W12', gt))
                        y1 = c_['y1']; y2 = c_['y2']; xt = c_['xt']
                        S.act(ya, y1, AF.Copy, scale=w12[:, 0:1])
                        S.stt(ya, y2, w12[:, 1:2], ya, ALU.mult, ALU.add)
                        S.tt('dve', ya, ya, G2[r], ALU.mult)
                        xn = xn_r.next()
                        S.tt('dve', xn, xt, ya, ALU.add)
                        if last:
                            S.act(junk, xn, AF.Square, accum=ssq)
                            rstd_from_ssq(rstd, ssq, tmp1, 1.0 / D)
                            ot = ot_r.next()
                            S.stt(ot, xn, rstd, fg, ALU.mult, ALU.mult)
                            S.st(out[b, (n - 2) * 128:(n - 1) * 128, :], ('out', b, n), ot)
                        else:
                            S.st(XS[b, n * 128:(n + 1) * 128, :], ('XS', b, n), xn)
                        del cx[gt]

                    pipeline(NG, [(f0, 2), (f1, 0)])
                S.barrier()
        S.barrier(engines=['sp'])
    return nc


def _consts():
    c = {}
    t = np.arange(T)
    rows = (t // 64).astype(np.float32)
    cols = (t % 64).astype(np.float32)
    inv = (10000.0 ** (-np.arange(32, dtype=np.float32) / 32)).astype(np.float32)
    ar = rows[:, None] * inv[None, :]
    ac = cols[:, None] * inv[None, :]
    C = np.concatenate([np.cos(ar), np.cos(ar), np.cos(ac), np.cos(ac)], axis=1)
    Sg = np.concatenate([-np.sin(ar), np.sin(ar), -np.sin(ac), np.sin(ac)], axis=1)
    c["c_rope_c"] = np.ascontiguousarray(C.reshape(16, 128, 128).transpose(1, 0, 2)).astype(np.float32)
    c["c_rope_s"] = np.ascontiguousarray(Sg.reshape(16, 128, 128).transpose(1, 0, 2)).astype(np.float32)
    band = np.zeros((128, 20, 128), np.float32)
    tp = np.arange(128)[:, None]
    tt = np.arange(128)[None, :]
    for g, w in enumerate((2, 4, 8, 16)):
        h = w // 2
        eye = (tp == tt).astype(np.float32)
        band[:, g * 5 + 0, :] = ((tp - 128) >= (tt - h)).astype(np.float32) / w
        band[:, g * 5 + 2, :] = ((tp + 128) <= (tt + h - 1)).astype(np.float32) / w
        inwin = ((tp >= tt - h) & (tp <= tt + h - 1)).astype(np.float32)
        band[:, g * 5 + 1, :] = inwin / w - eye
        cnt_first = (np.minimum(tt + h, 10 ** 9) - np.maximum(tt - h, 0)).astype(np.float32)
        band[:, g * 5 + 3, :] = inwin / cnt_first - eye
        cnt_last = (np.minimum(tt + h, 128) - (tt - h)).astype(np.float32)
        band[:, g * 5 + 4, :] = inwin / cnt_last - eye
    c["c_band"] = band
    c["c_ident"] = np.eye(128, dtype=np.float32)
    c["c_utri"] = (tp < tt).astype(np.float32)
    j = np.arange(128)[:, None].astype(np.float32)
    i = np.arange(128)[None, :].astype(np.float32)
    mt = np.zeros((128, 4, 128), np.float32)
    mt[:, 0, :] = np.maximum(i - j, 0)
    mt[:, 1, :] = (i >= j)
    mt[:, 2, :] = np.maximum(j - i, 0)
    mt[:, 3, :] = (j >= i)
    c["c_mtab"] = mt
    p = np.arange(128, dtype=np.float32)
    pc = np.zeros((128, 8), np.float32)
    pc[:, 0] = p + 1
    pc[:, 1] = 128 - p
    pc[:, 2] = 127 - p
    pc[:, 3] = p
    pc[:, 4] = 128
    c["c_pcols"] = pc
    c["c_blk"] = np.tile((np.arange(NBLK, dtype=np.float32) * 128)[None, :], (128, 1))
    return c


_NC_CACHE = {}


def _prep_inputs(inputs):
    f = lambda a: np.ascontiguousarray(np.asarray(a, dtype=np.float32))
    x = f(inputs["x"])
    ctx = f(inputs["ctx"])
    c = f(inputs["c"])
    c_ctx = f(inputs["c_ctx"])
    shared = {k: f(inputs[k]) for k in ("ada_w", "ada_b", "norm1_g", "w_in", "decay_fwd", "decay_bwd", "pool_w",
                                        "pool_scale", "w_out", "norm2_g", "exp_w_gate", "exp_w_up", "exp_w_down",
                                        "final_norm_g")}
    shared["router_w"] = np.ascontiguousarray(np.concatenate([f(inputs["router_g_w"]), f(inputs["router_e_w"])], axis=2))
    shared["router_b"] = np.ascontiguousarray(np.concatenate([f(inputs["router_g_b"]), f(inputs["router_e_b"])], axis=1))
    shared.update(_consts())
    in_maps = []
    for i in range(8):
        m = dict(shared)
        m["x"] = np.ascontiguousarray(x[2 * i:2 * i + 2])
        m["ctx"] = np.ascontiguousarray(ctx[2 * i:2 * i + 2])
        m["cvec"] = np.ascontiguousarray(np.stack([c[2 * i], c[2 * i + 1], c_ctx], axis=0))
        in_maps.append(m)
    return in_maps


def kernel(**inputs):
    in_maps = _prep_inputs(inputs)
    if "nc" not in _NC_CACHE:
        _NC_CACHE["nc"] = build()
    nc = _NC_CACHE["nc"]
    res = run_bass_kernel_spmd(nc, in_maps, core_ids=list(range(8)))
    return np.concatenate([np.asarray(r["out"], dtype=np.float32) for r in res.results], axis=0)
```
